# Optimizing a Trainium2 kernel written in Bass

```python
import jax, jax.numpy as jnp
from jax import lax
import numpy as np

D_MODEL = 1024
BATCH = 8
SEQ = 4096
DEPTH = 2

CTX_LEN = 256
GRID_W = 64
EPS = 1e-6
M_HEADS = 4
M_HEAD_DIM = D_MODEL // M_HEADS
M_WIDTH = M_HEADS * M_HEAD_DIM
M_CHUNK = 64
QK_CONV = 3
POOL_WINDOWS = (2, 4, 8, 16)
POOL_GROUPS = len(POOL_WINDOWS)
POOL_WIDTH = D_MODEL // 2
POOL_GROUP_DIM = POOL_WIDTH // POOL_GROUPS
D_FF = 2816
N_EXPERTS = 8
TOP_K = 2
D_FF_EXPERT = 3584
N_DENSE = (DEPTH + 1) // 2
N_MOE = DEPTH // 2
Q_OFF = 0
K_OFF = M_WIDTH
V_OFF = 2 * M_WIDTH
O_OFF = 3 * M_WIDTH
IF_OFF = 4 * M_WIDTH
N_GATE_COLS = 2 * 2 * M_HEADS
POOL_OFF = IF_OFF + N_GATE_COLS
GA_OFF = POOL_OFF + POOL_WIDTH
GB_OFF = GA_OFF + D_MODEL
IN_COLS = GB_OFF + D_MODEL

kernel_name = 'hybrid_mlstm_pool_moe_dit'


def _rmsnorm(x, g):
    xf = x.astype(jnp.float32)
    r = lax.rsqrt(jnp.mean(xf * xf, axis=-1, keepdims=True) + EPS)
    return (xf * r).astype(x.dtype) * g


def _dwconv_centred(x, w):
    k = w.shape[0]
    pad = k // 2
    s = x.shape[1]
    xp = jnp.pad(x, ((0, 0), (pad, pad), (0, 0)))
    out = xp[:, 0:s] * w[0]
    for j in range(1, k):
        out = out + xp[:, j:j + s] * w[j]
    return out


def _split_heads(t):
    b, s, _ = t.shape
    return t.reshape(b, s, M_HEADS, M_HEAD_DIM).transpose(0, 2, 1, 3)


def _mlstm_chunkwise(q, k, v, ig, lf, state, with_outputs):
    b_, h_, s_, dh = q.shape
    nc = s_ // M_CHUNK

    def chunks(t):
        t = t.reshape(b_, h_, nc, M_CHUNK, *t.shape[3:])
        return jnp.moveaxis(t, 2, 0)

    lower = jnp.tril(jnp.ones((M_CHUNK, M_CHUNK), dtype=bool))

    def step(carry, xs):
        c_st, n_st, m_st = carry
        qc, kc, vc, ic, fc = xs
        bcum = jnp.cumsum(fc, axis=-1)
        b_last = bcum[..., -1]
        g_end = b_last[..., None] - bcum + ic
        m_new = jnp.maximum(b_last + m_st, jnp.max(g_end, axis=-1))
        wk = jnp.exp(g_end - m_new[..., None])
        wc = jnp.exp(b_last + m_st - m_new)
        c_new = wc[..., None, None] * c_st + jnp.einsum('bhs,bhsd,bhse->bhde', wk, kc, vc)
        n_new = wc[..., None] * n_st + jnp.einsum('bhs,bhsd->bhd', wk, kc)
        if not with_outputs:
            return (c_new, n_new, m_new), None
        a_inter = bcum + m_st[..., None]
        dmat = jnp.where(lower, bcum[..., :, None] - bcum[..., None, :] + ic[..., None, :], -jnp.inf)
        m_row = jnp.maximum(a_inter, jnp.max(dmat, axis=-1))
        w_inter = jnp.exp(a_inter - m_row)
        scores = jnp.einsum('bhjd,bhsd->bhjs', qc, kc) * jnp.exp(dmat - m_row[..., None])
        num = (w_inter[..., None] * jnp.einsum('bhjd,bhde->bhje', qc, c_st)
               + jnp.einsum('bhjs,bhse->bhje', scores, vc))
        den = w_inter * jnp.einsum('bhjd,bhd->bhj', qc, n_st) + jnp.sum(scores, axis=-1)
        hc = num / jnp.maximum(jnp.abs(den), jnp.exp(-m_row))[..., None]
        return (c_new, n_new, m_new), hc

    state, hs = lax.scan(step, state, (chunks(q), chunks(k), chunks(v), chunks(ig), chunks(lf)))
    if hs is not None:
        hs = jnp.moveaxis(hs, 0, 2).reshape(b_, h_, s_, dh)
    return state, hs


def _box_mean(x, axis, w):
    n = x.shape[axis]
    cs = jnp.cumsum(x, axis=axis)
    zero = jnp.zeros_like(lax.slice_in_dim(cs, 0, 1, axis=axis))
    cs = jnp.concatenate([zero, cs], axis=axis)
    t = jnp.arange(n)
    lo = jnp.clip(t - w // 2, 0, n)
    hi = jnp.clip(t + w // 2, 0, n)
    total = jnp.take(cs, hi, axis=axis) - jnp.take(cs, lo, axis=axis)
    shape = [1] * x.ndim
    shape[axis] = n
    return total / (hi - lo).astype(x.dtype).reshape(shape)


def _multiscale_pool(p, rows):
    b, s, _ = p.shape
    pf = p.astype(jnp.float32)
    outs = []
    for g, w in enumerate(POOL_WINDOWS):
        xg = pf[..., g * POOL_GROUP_DIM:(g + 1) * POOL_GROUP_DIM]
        if rows is None:
            pooled = _box_mean(xg, 1, w)
        else:
            grid = xg.reshape(b, rows, GRID_W, POOL_GROUP_DIM)
            pooled = _box_mean(_box_mean(grid, 2, w), 1, w).reshape(b, s, POOL_GROUP_DIM)
        outs.append(pooled - xg)
    return jnp.concatenate(outs, axis=-1).astype(p.dtype)


def _mixer(u, w_in, conv_qk, b_if, head_gain, w_pool, pool_scale, w_pa, w_pb, w_out,
           init_f, init_b, rows, with_outputs):
    b, s, _ = u.shape
    f32 = jnp.float32
    z = u @ w_in
    qk = jax.nn.silu(_dwconv_centred(z[..., Q_OFF:V_OFF], conv_qk))
    q = _split_heads(qk[..., :M_WIDTH]).astype(f32)
    k = _split_heads(qk[..., M_WIDTH:]).astype(f32) * (M_HEAD_DIM ** -0.5)
    v = _split_heads(z[..., V_OFF:O_OFF]).astype(f32)
    gates = z[..., IF_OFF:POOL_OFF].astype(f32).reshape(b, s, 2, 2, M_HEADS) + b_if.astype(f32)
    gates = gates.transpose(2, 3, 0, 4, 1)
    flip = lambda t: jnp.flip(t, axis=2)
    st_f, h_f = _mlstm_chunkwise(q, k, v, gates[0, 0], jax.nn.log_sigmoid(gates[0, 1]),
                                 init_f, with_outputs)
    st_b, h_b = _mlstm_chunkwise(flip(q), flip(k), flip(v), flip(gates[1, 0]),
                                 flip(jax.nn.log_sigmoid(gates[1, 1])), init_b, with_outputs)
    if not with_outputs:
        return None, st_f, st_b
    hm = h_f + flip(h_b)
    mu = jnp.mean(hm, axis=-1, keepdims=True)
    var = jnp.mean(jnp.square(hm - mu), axis=-1, keepdims=True)
    hm = ((hm - mu) * lax.rsqrt(var + EPS)).transpose(0, 2, 1, 3).reshape(b, s, M_WIDTH).astype(u.dtype)
    hm = jax.nn.sigmoid(z[..., O_OFF:IF_OFF]) * (hm * head_gain)
    pm = _multiscale_pool(z[..., POOL_OFF:GA_OFF], rows)
    pm = jnp.einsum('bsgc,gce->bsge', pm.reshape(b, s, POOL_GROUPS, POOL_GROUP_DIM), w_pool)
    pm = pm.reshape(b, s, POOL_WIDTH) * pool_scale
    merged = (jax.nn.sigmoid(z[..., GA_OFF:GB_OFF]) * (hm @ w_pa)
              + jax.nn.sigmoid(z[..., GB_OFF:IN_COLS]) * (pm @ w_pb))
    return merged @ w_out, st_f, st_b


def _swiglu(t, wg, wu, wd):
    return (jax.nn.silu(t @ wg) * (t @ wu)) @ wd


def _moe(u, w_router, w_g, w_u, w_d):
    b, s, d = u.shape
    t = u.reshape(b * s, d)
    logits = (t @ w_router).astype(jnp.float32)
    top_val, top_idx = lax.top_k(logits, TOP_K)
    probs = jax.nn.softmax(top_val, axis=-1)
    gate = jnp.sum(jax.nn.one_hot(top_idx, N_EXPERTS, dtype=jnp.float32) * probs[..., None], axis=1)
    gate = gate.astype(t.dtype)
    y = jnp.zeros_like(t)
    for e in range(N_EXPERTS):
        y = y + gate[:, e:e + 1] * _swiglu(t, w_g[e], w_u[e], w_d[e])
    return y.reshape(b, s, d)


def _channel_mixer(l, u, w_ff_gate, w_ff_up, w_ff_down, w_router, w_exp_gate, w_exp_up, w_exp_down):
    j = l // 2
    if l % 2 == 0:
        return _swiglu(u, w_ff_gate[j], w_ff_up[j], w_ff_down[j])
    return _moe(u, w_router[j], w_exp_gate[j], w_exp_up[j], w_exp_down[j])


def setup_inputs(seed: int = 0) -> dict:
    key = jax.random.key(seed)
    ks = jax.random.split(key, 26)
    nrm = jax.random.normal
    f32 = jnp.float32
    d = D_MODEL
    return {
        'x': nrm(ks[0], (BATCH, SEQ, d), f32),
        'c': nrm(ks[1], (BATCH, d), f32),
        'ctx': nrm(ks[2], (BATCH, CTX_LEN, d), f32),
        'c_ctx': nrm(ks[3], (d,), f32),
        'w_ada': nrm(ks[4], (DEPTH, d, 6 * d), f32) * (0.5 * d ** -0.5),
        'b_ada': 0.02 * nrm(ks[5], (DEPTH, 6 * d), f32),
        'g_mix': 1.0 + 0.1 * nrm(ks[6], (DEPTH, d), f32),
        'w_in': nrm(ks[7], (DEPTH, d, IN_COLS), f32) * d ** -0.5,
        'conv_qk': nrm(ks[8], (DEPTH, QK_CONV, 2 * M_WIDTH), f32) * QK_CONV ** -0.5,
        'b_if': jnp.array([-1.0, 3.0], f32)[None, None, :, None] + 0.3 * nrm(ks[9], (DEPTH, 2, 2, M_HEADS), f32),
        'head_gain': 1.0 + 0.1 * nrm(ks[10], (DEPTH, M_WIDTH), f32),
        'w_pool': nrm(ks[11], (DEPTH, POOL_GROUPS, POOL_GROUP_DIM, POOL_GROUP_DIM), f32) * POOL_GROUP_DIM ** -0.5,
        'pool_scale': 1.0 + 0.1 * nrm(ks[12], (DEPTH, POOL_WIDTH), f32),
        'w_pa': nrm(ks[13], (DEPTH, M_WIDTH, d), f32) * M_WIDTH ** -0.5,
        'w_pb': nrm(ks[14], (DEPTH, POOL_WIDTH, d), f32) * POOL_WIDTH ** -0.5,
        'w_out': nrm(ks[15], (DEPTH, d, d), f32) * d ** -0.5,
        'g_ffn': 1.0 + 0.1 * nrm(ks[16], (DEPTH, d), f32),
        'w_ff_gate': nrm(ks[17], (N_DENSE, d, D_FF), f32) * d ** -0.5,
        'w_ff_up': nrm(ks[18], (N_DENSE, d, D_FF), f32) * d ** -0.5,
        'w_ff_down': nrm(ks[19], (N_DENSE, D_FF, d), f32) * D_FF ** -0.5,
        'w_router': nrm(ks[20], (N_MOE, d, N_EXPERTS), f32) * d ** -0.5,
        'w_exp_gate': nrm(ks[21], (N_MOE, N_EXPERTS, d, D_FF_EXPERT), f32) * d ** -0.5,
        'w_exp_up': nrm(ks[22], (N_MOE, N_EXPERTS, d, D_FF_EXPERT), f32) * d ** -0.5,
        'w_exp_down': nrm(ks[23], (N_MOE, N_EXPERTS, D_FF_EXPERT, d), f32) * D_FF_EXPERT ** -0.5,
        'g_final': 1.0 + 0.1 * nrm(ks[24], (d,), f32),
    }


def reference(x, c, ctx, c_ctx, w_ada, b_ada, g_mix, w_in, conv_qk, b_if, head_gain, w_pool, pool_scale,
              w_pa, w_pb, w_out, g_ffn, w_ff_gate, w_ff_up, w_ff_down, w_router, w_exp_gate, w_exp_up,
              w_exp_down, g_final):
    rows = x.shape[1] // GRID_W
    bsz = ctx.shape[0]
    f32 = jnp.float32
    zero_state = (jnp.zeros((bsz, M_HEADS, M_HEAD_DIM, M_HEAD_DIM), f32),
                  jnp.zeros((bsz, M_HEADS, M_HEAD_DIM), f32),
                  jnp.zeros((bsz, M_HEADS), f32))
    h = x
    h_ctx = ctx
    ffn_w = (w_ff_gate, w_ff_up, w_ff_down, w_router, w_exp_gate, w_exp_up, w_exp_down)
    for l in range(DEPTH):
        last = l == DEPTH - 1
        mod = jax.nn.silu(c) @ w_ada[l] + b_ada[l]
        mod_c = jax.nn.silu(c_ctx) @ w_ada[l] + b_ada[l]
        sh1, sc1, gt1, sh2, sc2, gt2 = jnp.split(mod[:, None, :], 6, axis=-1)
        sh1c, sc1c, gt1c, sh2c, sc2c, gt2c = jnp.split(mod_c, 6, axis=-1)
        mix_w = (w_in[l], conv_qk[l], b_if[l], head_gain[l], w_pool[l], pool_scale[l],
                 w_pa[l], w_pb[l], w_out[l])
        u_c = _rmsnorm(h_ctx, g_mix[l]) * (1.0 + sc1c) + sh1c
        y_c, st_f, st_b = _mixer(u_c, *mix_w, zero_state, zero_state, None, not last)
        u_x = _rmsnorm(h, g_mix[l]) * (1.0 + sc1) + sh1
        y_x, _, _ = _mixer(u_x, *mix_w, st_f, st_b, rows, True)
        h = h + gt1 * y_x
        u_x = _rmsnorm(h, g_ffn[l]) * (1.0 + sc2) + sh2
        h = h + gt2 * _channel_mixer(l, u_x, *ffn_w)
        if not last:
            h_ctx = h_ctx + gt1c * y_c
            u_c = _rmsnorm(h_ctx, g_ffn[l]) * (1.0 + sc2c) + sh2c
            h_ctx = h_ctx + gt2c * _channel_mixer(l, u_c, *ffn_w)
    return _rmsnorm(h, g_final)
```

```python
import numpy as np
import concourse.bass as bass
import concourse.mybir as mybir
from concourse.bass_utils import run_bass_kernel_spmd

F32 = mybir.dt.float32
BF16 = mybir.dt.bfloat16
AF = mybir.ActivationFunctionType
ALU = mybir.AluOpType

EPOCH = 20000
N_DMA_TL = 12


class Buf:
    __slots__ = ("name", "last_writer", "readers", "readers_d")

    def __init__(self, name):
        self.name = name
        self.last_writer = None
        self.readers = {}
        self.readers_d = []


class Op:
    __slots__ = ("eng", "fn", "reads", "writes", "dma", "deps", "signal", "sig", "id", "barrier")


class Prog:
    ENGS = ("pe", "act", "dve", "pool", "sp")

    def __init__(self, nc):
        self.nc = nc
        self.ops = []
        self.bufs = {}
        self._uid = 0

    def buf(self, *key):
        b = self.bufs.get(key)
        if b is None:
            b = Buf(key)
            self.bufs[key] = b
        return b

    def newbuf(self, tag="b"):
        self._uid += 1
        return self.buf(tag, self._uid)

    def op(self, eng, fn, reads=(), writes=(), dma=False):
        o = Op()
        o.eng = eng
        o.fn = fn
        o.reads = tuple(reads)
        o.writes = tuple(writes)
        o.dma = dma
        o.id = len(self.ops)
        o.signal = dma
        o.sig = None
        o.barrier = False
        o.deps = ()
        self.ops.append(o)
        return o

    def barrier(self):
        o = self.op(None, None)
        o.barrier = True
        return o

    def dma(self, eng, out, in_, reads=(), writes=(), **kw):
        return self.op(eng, lambda e: e.dma_start(out=out, in_=in_, **kw), reads, writes, dma=True)

    def finalize(self):
        nc = self.nc
        ops = self.ops
        last_on = {e: None for e in self.ENGS}
        for o in ops:
            if o.barrier:
                for e in self.ENGS:
                    if last_on[e] is not None:
                        ops[last_on[e]].signal = True
                for b in self.bufs.values():
                    b.last_writer = None
                    b.readers = {}
                    b.readers_d = []
                continue
            deps = set()
            for b in o.reads:
                if b.last_writer is not None:
                    deps.add(b.last_writer)
            for b in o.writes:
                if b.last_writer is not None:
                    deps.add(b.last_writer)
                deps.update(b.readers.values())
                deps.update(b.readers_d)
            for b in o.writes:
                b.last_writer = o.id
                b.readers = {}
                b.readers_d = []
            for b in o.reads:
                if b.last_writer != o.id:
                    if o.dma:
                        b.readers_d.append(o.id)
                    else:
                        b.readers[o.eng] = o.id
            deps.discard(o.id)
            fd = []
            for d in deps:
                p = ops[d]
                if (not p.dma) and (not o.dma) and p.eng == o.eng and o.eng == "pe":
                    continue
                fd.append(d)
            o.deps = fd
            for d in fd:
                ops[d].signal = True
            if not o.dma:
                last_on[o.eng] = o.id
        cnt = {e: 0 for e in self.ENGS}
        dma_rr = {e: 0 for e in self.ENGS}
        dma_uses = {}
        for o in ops:
            if o.barrier:
                continue
            if o.dma:
                k = dma_rr[o.eng] % N_DMA_TL
                dma_rr[o.eng] += 1
                tl = ("dma", o.eng, k)
                dma_uses[tl] = dma_uses.get(tl, 0) + 1
                o.sig = (tl, dma_uses[tl])
            elif o.signal:
                cnt[o.eng] += 1
                o.sig = (("eng", o.eng), cnt[o.eng])
        sems = {}
        for e in self.ENGS:
            n_ep = (cnt[e] + EPOCH - 1) // EPOCH
            for ep in range(max(n_ep, 1)):
                sems[(("eng", e), ep)] = nc.alloc_semaphore(f"s_{e}_{ep}")
        for tl in dma_uses:
            sems[(tl, 0)] = nc.alloc_semaphore(f"s_dma_{tl[1]}_{tl[2]}")

        def sem_of(tl, v):
            if tl[0] == "eng":
                ep = (v - 1) // EPOCH
                return sems[(tl, ep)], v - ep * EPOCH
            return sems[(tl, 0)], v * 16

        seen = {e: {} for e in self.ENGS}
        pending = {e: None for e in self.ENGS}
        cur = {}
        streams = {e: [] for e in self.ENGS}
        n_waits = 0
        for o in ops:
            if o.barrier:
                snap = dict(cur)
                for e in self.ENGS:
                    if pending[e] is None:
                        pending[e] = dict(snap)
                    else:
                        pending[e].update(snap)
                continue
            E = o.eng
            need = {}
            if pending[E] is not None:
                need.update(pending[E])
                pending[E] = None
            for d in o.deps:
                tl, v = ops[d].sig
                if need.get(tl, 0) < v:
                    need[tl] = v
            if o.dma:
                tl, v = o.sig
                if v > 1 and need.get(tl, 0) < v - 1:
                    need[tl] = v - 1
            waits = []
            for tl, v in need.items():
                if seen[E].get(tl, 0) < v:
                    seen[E][tl] = v
                    waits.append(sem_of(tl, v))
            n_waits += len(waits)
            inc = None
            if o.sig is not None:
                tl, v = o.sig
                cur[tl] = v
                s, _ = sem_of(tl, v)
                inc = (s, 16 if tl[0] == "dma" else 1)
            streams[E].append((waits, o.fn, inc))
        fin = []
        for tl, v in cur.items():
            fin.append(sem_of(tl, v))
        self.stats = dict(n_ops=len(ops), n_waits=n_waits, cnt=dict(cnt), n_dma=sum(dma_uses.values()),
                          n_sems=len(sems))
        engmap = {"pe": "tensor", "act": "scalar", "dve": "vector", "pool": "gpsimd", "sp": "sync"}
        with nc.Block() as block:
            for e in self.ENGS:
                lst = streams[e]
                is_sp = e == "sp"

                def body(eng, lst=lst, is_sp=is_sp):
                    for waits, fn, inc in lst:
                        for s, v in waits:
                            eng.wait_ge(s, v)
                        ins = fn(eng)
                        if inc is not None:
                            ins.then_inc(inc[0], inc[1])
                    if is_sp:
                        for s, v in fin:
                            eng.wait_ge(s, v)

                if lst or is_sp:
                    getattr(block, engmap[e])(body)
        return self.stats


class T:
    __slots__ = ("t", "b")

    def __init__(self, t, b):
        self.t = t
        self.b = b

    def __getitem__(self, k):
        return self.t[k]


class SBA:
    BASE = 16512
    END = 229376

    def __init__(self, nc, P):
        self.nc = nc
        self.P = P
        self.lo = self.BASE
        self.n = 0

    def alloc(self, shape, dtype):
        sz = 4 if dtype == F32 else 2
        n = sz
        for s in shape[1:]:
            n *= s
        n = (n + 63) // 64 * 64
        off = self.lo
        self.lo += n
        assert self.lo <= self.END, f"SBUF overflow {self.lo}"
        self.peak = max(getattr(self, "peak", 0), self.lo)
        self.n += 1
        t = self.nc.alloc_sbuf_tensor_at(f"sb{self.n}", list(shape), dtype, offset=off)
        return T(t, self.P.newbuf("sb"))

    def mark(self):
        return self.lo

    def release(self, m):
        self.lo = m


class Rot:
    def __init__(self, items):
        self.items = items
        self.i = 0

    def next(self):
        it = self.items[self.i % len(self.items)]
        self.i += 1
        return it


D = 1024
SEQ = 4096
CTX = 256
NH = 4
DH = 256
DFF = 2816
NE = 8
DFE = 3584
EPS = 1e-6
IN_COLS = 6672
POOL_W = (2, 4, 8, 16)
Q_OFF, K_OFF, V_OFF, O_OFF, IF_OFF = 0, 1024, 2048, 3072, 4096
POOL_OFF = 4112
GA_OFF = POOL_OFF + 512
GB_OFF = GA_OFF + 1024
NEG = -30000.0


def build(debug=None):
    nc = bass.Bass("TRN2", target_bir_lowering=False)
    P = Prog(nc)
    SB = SBA(nc, P)

    def din(name, shape, dt=F32):
        return nc.dram_tensor(name, list(shape), dt, kind="ExternalInput").ap()

    debug = debug or {}
    dbg_names = set(debug.get("names", ()))

    def dscr(name, shape, dt):
        kind = "ExternalOutput" if name in dbg_names else "Internal"
        return nc.dram_tensor(name, list(shape), dt, kind=kind).ap()

    x_in = din("x", [SEQ, D])
    ctx_in = din("ctx", [CTX, D])
    cc_in = din("cc", [128, 8, 2])
    w_ada = din("w_ada", [2, D, 6 * D])
    b_ada = din("b_ada", [2, 1, 6 * D])
    vecs_in = din("vecs", [2, 128, 28])
    convw_in = din("convw", [2, 128, 16, 3])
    w_in = din("w_in", [2, D, IN_COLS])
    w_if = din("w_if", [2, D, 16])
    b_if = din("b_if", [2, 1, 16])
    w_pool = din("w_pool", [2, 4, 128, 128])
    w_pa = din("w_pa", [2, D, D])
    w_pb = din("w_pb", [2, 512, D])
    w_out = din("w_out", [2, D, D])
    w_ffg = din("w_ff_gate", [1, D, DFF])
    w_ffu = din("w_ff_up", [1, D, DFF])
    w_ffd = din("w_ff_down", [1, DFF, D])
    w_router = din("w_router", [1, D, NE])
    w_eg = din("w_exp_gate", [1, NE, D, DFE])
    w_eu = din("w_exp_up", [1, NE, D, DFE])
    w_ed = din("w_exp_down", [1, NE, DFE, D])
    gfin_in = din("g_final_b", [128, D])
    cmat_in = din("cmat", [6, 128, 128])
    invc_lat = din("invc_lat", [4, 128, SEQ])
    invc_ctx = din("invc_ctx", [4, 128, CTX])
    y_out = nc.dram_tensor("y", [SEQ, D], F32, kind="ExternalOutput").ap()

    def mk_scr(sfx, NT):
        s = {}
        s["qT"] = dscr("qT" + sfx, [D, NT], BF16)
        s["kT"] = dscr("kT" + sfx, [D, NT], BF16)
        s["ktm"] = dscr("ktm" + sfx, [NT, D], BF16)
        s["vtm"] = dscr("vtm" + sfx, [NT, D], BF16)
        s["oT"] = dscr("oT" + sfx, [D, NT], BF16)
        s["gaT"] = dscr("gaT" + sfx, [D, NT], BF16)
        s["gbT"] = dscr("gbT" + sfx, [D, NT], BF16)
        s["pmwT"] = dscr("pmwT" + sfx, [512, NT], BF16)
        s["hf"] = dscr("hf" + sfx, [NT, D], F32)
        s["u2T"] = dscr("u2T" + sfx, [D, NT], BF16)
        s["h1"] = dscr("h1" + sfx, [NT, D], F32)
        s["h2"] = dscr("h2" + sfx, [NT, D], F32)
        return s

    scr_x = mk_scr("_x", SEQ)
    scr_c = mk_scr("_c", CTX)
    B = P.buf

    banks = []
    for i in range(8):
        banks.append(T(nc.alloc_psum_tensor(f"ps{i}", [128, 512], F32), B("bank", i)))
    bankrot = Rot(banks)

    def rb(*ts):
        return [t.b for t in ts]

    def ACT(out, in_, func, reads, writes, **kw):
        P.op("act", lambda e: e.activation(out=out, in_=in_, func=func, **kw), reads, writes)

    def TS(eng, out, in0, s1, s2, op0, op1, reads, writes):
        if op1 is None:
            P.op(eng, lambda e: e.tensor_scalar(out=out, in0=in0, scalar1=s1, scalar2=None, op0=op0), reads, writes)
        else:
            P.op(eng, lambda e: e.tensor_scalar(out=out, in0=in0, scalar1=s1, scalar2=s2, op0=op0, op1=op1), reads, writes)

    def TT_(eng, out, in0, in1, op, reads, writes):
        P.op(eng, lambda e: e.tensor_tensor(out=out, in0=in0, in1=in1, op=op), reads, writes)

    def STT(eng, out, in0, scalar, in1, op0, op1, reads, writes):
        P.op(eng, lambda e: e.scalar_tensor_tensor(out=out, in0=in0, scalar=scalar, in1=in1, op0=op0, op1=op1), reads, writes)

    def CP(eng, out, in_, reads, writes):
        if eng == "act":
            P.op(eng, lambda e: e.activation(out=out, in_=in_, func=AF.Identity), reads, writes)
        else:
            P.op(eng, lambda e: e.tensor_copy(out=out, in_=in_), reads, writes)

    def MS(eng, ap, val, writes):
        P.op(eng, lambda e: e.memset(ap, val), (), writes)

    def MM(bank, out, lhsT, rhs, start, stop, reads):
        P.op("pe", lambda e: e.matmul(out, lhsT=lhsT, rhs=rhs, start=start, stop=stop), reads, [bank.b])

    def TR(bank, out, in_, ident, reads):
        P.op("pe", lambda e: e.transpose(out=out, in_=in_, identity=ident), reads, [bank.b])

    def LD(dst_ap, src_ap, tile, q="sp", reads=(), **kw):
        P.dma(q, dst_ap, src_ap, reads=reads, writes=[tile.b], **kw)

    def ST(dst_ap, src_ap, tile, dbuf, q="sp"):
        P.dma(q, dst_ap, src_ap, reads=[tile.b], writes=[dbuf])

    cm = SB.alloc([128, 6, 128], F32)
    LD(cm[:], cmat_in.rearrange("c p n -> p c n"), cm)
    ident = cm[:, 0, :]
    tri = (cm[:, 1, :], cm[:, 2, :])
    negm = (cm[:, 3, :], cm[:, 4, :])
    ones = cm[:, 5, :]
    epsc = SB.alloc([128, 4], F32)
    MS("dve", epsc[:, 0:1], EPS, [epsc.b])
    MS("dve", epsc[:, 1:2], 1.0, [epsc.b])
    MS("dve", epsc[:, 2:3], float(-np.log(16.0)), [epsc.b])
    MS("dve", epsc[:, 3:4], 0.0, [epsc.b])
    scc = SB.alloc([128, 8, 2], F32)
    LD(scc[:], cc_in, scc)
    ACT(scc[:], scc[:], AF.Silu, [], [scc.b])
    m_persist = SB.mark()

    def adaln(l, want_ctx_gt):
        lay = {}
        modT = SB.alloc([128, 48, 2], F32)
        vecs = SB.alloc([128, 28], F32)
        LD(vecs[:], vecs_in[l], vecs)
        convw = SB.alloc([128, 16, 3], F32)
        LD(convw[:], convw_in[l], convw)
        s1 = SB.alloc([128, 8, 2], F32)
        s2 = SB.alloc([128, 8, 2], F32)
        gtb = {}
        for nm in (("gt1x", "gt2x", "gt1c", "gt2c") if want_ctx_gt else ("gt1x", "gt2x")):
            gtb[nm] = SB.alloc([128, D], F32)
        m0 = SB.mark()
        brow = SB.alloc([1, 6 * D], F32)
        LD(brow[:], b_ada[l], brow)
        rep = SB.alloc([128, 2, 8, 128], F32)
        for v in range(2):
            for k in range(8):
                TS("dve", rep[:, v, k, :], ones, scc[:, k, v:v + 1], None, ALU.mult, None, [cm.b, scc.b], [rep.b])
        wblk = [SB.alloc([128, 8, 512], F32) for _ in range(2)]
        for j in range(12):
            wb = wblk[j % 2]
            LD(wb[:], w_ada[l, :, j * 512:(j + 1) * 512].rearrange("(k p) n -> p k n", p=128), wb)
            bank = bankrot.next()
            for c in range(4):
                for k in range(8):
                    MM(bank, bank[:, c * 2:c * 2 + 2], wb[:, k, c * 128:(c + 1) * 128], scc[:, k, :], k == 0, False,
                       [wb.b, scc.b])
                MM(bank, bank[:, c * 2:c * 2 + 2], brow[0:1, j * 512 + c * 128:j * 512 + (c + 1) * 128], ones[0:1, 0:2],
                   False, True, [brow.b, cm.b])
            CP("dve", modT[:, j * 4:(j + 1) * 4, :], bank[:, 0:8].rearrange("p (c v) -> p c v", v=2), [], [bank.b, modT.b])
            which = {4: ("gt1", 0), 5: ("gt1", 1), 10: ("gt2", 0), 11: ("gt2", 1)}.get(j)
            if which is not None:
                for v, sfx in ((0, "x"), (1, "c")):
                    nm = which[0] + sfx
                    if nm not in gtb:
                        continue
                    bank = bankrot.next()
                    for k in range(8):
                        MM(bank, bank[:, :], rep[:, v, k, :], wb[:, k, :], k == 0, False, [rep.b, wb.b])
                    MM(bank, bank[:, :], ones[0:1, :], brow[0:1, j * 512:(j + 1) * 512], False, True, [cm.b, brow.b])
                    CP("act", gtb[nm][:, which[1] * 512:(which[1] + 1) * 512], bank[:, :], [], [bank.b, gtb[nm].b])
        for v in range(2):
            TS("dve", s1[:, :, v], modT[:, 8:16, v], 1.0, None, ALU.add, None, [modT.b], [s1.b])
            TT_("dve", s1[:, :, v], s1[:, :, v], vecs[:, 0:8], ALU.mult, [s1.b, vecs.b], [s1.b])
            TS("dve", s2[:, :, v], modT[:, 32:40, v], 1.0, None, ALU.add, None, [modT.b], [s2.b])
            TT_("dve", s2[:, :, v], s2[:, :, v], vecs[:, 16:24], ALU.mult, [s2.b, vecs.b], [s2.b])
        P.barrier()
        SB.release(m0)
        lay.update(modT=modT, vecs=vecs, convw=convw, s1=s1, s2=s2, gtb=gtb)
        return lay

    def norm_transpose(src_tile, reads_src, sc_ap, bi_ap, out_fn, tmp):
        ss, xn = tmp
        ACT(xn[:], src_tile, AF.Square, reads_src, [xn.b, ss.b], accum_out=ss[:, 0:1])
        ACT(ss[:, 1:2], ss[:, 0:1], AF.Sqrt, [epsc.b], [ss.b], scale=1.0 / D, bias=epsc[:, 0:1])
        P.op("dve", lambda e: e.reciprocal(out=ss[:, 1:2], in_=ss[:, 1:2]), [], [ss.b])
        TS("dve", xn[:], src_tile, ss[:, 1:2], None, ALU.mult, None, list(reads_src) + [ss.b], [xn.b])
        for hf in range(debug.get("nhf", 2)):
            bank = bankrot.next()
            for i in range(4):
                k = hf * 4 + i
                TR(bank, bank[:, i * 128:(i + 1) * 128], xn[:, k * 128:(k + 1) * 128], ident, [xn.b, cm.b])
            for i in range(4):
                k = hf * 4 + i
                o_ap, o_bufs = out_fn(k)
                sc, scb = sc_ap(k)
                bi, bib = bi_ap(k)
                if i % 2 == 0:
                    ACT(o_ap, bank[:, i * 128:(i + 1) * 128], AF.Identity, [scb, bib], [bank.b] + list(o_bufs), scale=sc, bias=bi)
                else:
                    TS("dve", o_ap, bank[:, i * 128:(i + 1) * 128], sc, bi, ALU.mult, ALU.add, [scb, bib], [bank.b] + list(o_bufs))

    def phase_proj(l, lay, seq, h_src):
        NT = seq["NT"]
        TT = NT // 128
        var = seq["var"]
        scr = seq["scr"]
        TK = min(512, NT)
        nTK = NT // TK
        mA = SB.mark()
        uT = SB.alloc([128, 8, NT], BF16)
        gsb = seq["gsb"]
        m1 = SB.mark()
        xts = Rot([SB.alloc([128, D], F32) for _ in range(3)])
        tmps = Rot([(SB.alloc([128, 2], F32), SB.alloc([128, D], F32)) for _ in range(2)])
        for t in range(TT if NT == CTX else debug.get("a1_tiles", TT)):
            xt = xts.next()
            LD(xt[:], h_src[t * 128:(t + 1) * 128, :], xt, reads=[seq["hbuf"]])
            norm_transpose(xt[:], [xt.b], lambda k: (lay["s1"][:, k, var:var + 1], lay["s1"].b),
                           lambda k: (lay["modT"][:, k, var:var + 1], lay["modT"].b),
                           lambda k, t=t: (uT[:, k, t * 128:(t + 1) * 128], [uT.b]), tmps.next())
        P.barrier()
        SB.release(m1)
        if debug.get("sub") == "A1" and NT == SEQ:
            return
        wrot = Rot([SB.alloc([128, 8, 512], BF16) for _ in range(2)])

        def load_w(col0, ncols=512):
            wb = wrot.next()
            LD(wb[:, :, 0:ncols], w_in[l, :, col0:col0 + ncols].rearrange("(k p) n -> p k n", p=128), wb, q="pool")
            return wb

        def fm_chunk(wb, c, evac):
            for tk in range(nTK):
                bank = bankrot.next()
                for k in range(8):
                    MM(bank, bank[:, 0:TK], wb[:, k, c * 128:(c + 1) * 128], uT[:, k, tk * TK:(tk + 1) * TK], k == 0, k == 7,
                       [wb.b, uT.b])
                evac(bank, tk)

        m2 = SB.mark()
        zcs = Rot([SB.alloc([128, NT + 2], F32) for _ in range(2)])
        for z in zcs.items:
            MS("dve", z[:], 0.0, [z.b])
        caccs = Rot([SB.alloc([128, NT], F32) for _ in range(2)])
        qkbfs = Rot([SB.alloc([128, NT], BF16) for _ in range(1)])
        ktsts = Rot([SB.alloc([128, 4, 128], BF16) for _ in range(2)])
        convw = lay["convw"]
        for blk in range(4):
            wb = load_w(blk * 512)
            for c in range(4):
                cg = blk * 4 + c
                zc = zcs.next()
                cacc = caccs.next()
                fm_chunk(wb, c, lambda bank, tk, zc=zc: CP("act", zc[:, 1 + tk * TK:1 + (tk + 1) * TK], bank[:, 0:TK], [],
                                                          [bank.b, zc.b]))
                TS("dve", cacc[:], zc[:, 1:NT + 1], convw[:, cg, 1:2], None, ALU.mult, None, [zc.b, convw.b], [cacc.b])
                STT("dve", cacc[:], zc[:, 0:NT], convw[:, cg, 0:1], cacc[:], ALU.mult, ALU.add, [zc.b, convw.b], [cacc.b])
                STT("dve", cacc[:], zc[:, 2:NT + 2], convw[:, cg, 2:3], cacc[:], ALU.mult, ALU.add, [zc.b, convw.b], [cacc.b])
                qb = qkbfs.next()
                if cg < 8:
                    ACT(qb[:], cacc[:], AF.Silu, [cacc.b], [qb.b])
                    ST(scr["qT"][cg * 128:(cg + 1) * 128, :], qb[:], qb, B("qT", var))
                else:
                    kc = cg - 8
                    ACT(cacc[:], cacc[:], AF.Silu, [], [cacc.b])
                    CP("dve", qb[:], cacc[:], [cacc.b], [qb.b])
                    ST(scr["kT"][kc * 128:(kc + 1) * 128, :], qb[:], qb, B("kT", var))
                    for t0 in range(0, TT, 4):
                        nt = min(4, TT - t0)
                        bank = bankrot.next()
                        for i in range(nt):
                            TR(bank, bank[:, i * 128:(i + 1) * 128], cacc[:, (t0 + i) * 128:(t0 + i + 1) * 128], ident,
                               [cacc.b, cm.b])
                        kst = ktsts.next()
                        CP("act", kst[:, 0:nt, :], bank[:, 0:nt * 128].rearrange("p (t d) -> p t d", d=128), [],
                           [bank.b, kst.b])
                        ST(scr["ktm"][t0 * 128:(t0 + nt) * 128, kc * 128:(kc + 1) * 128].rearrange("(t p) d -> p t d", p=128),
                           kst[:, 0:nt, :], kst, B("ktm", var))
        P.barrier()
        SB.release(m2)
        if debug.get("sub") == "A2a" and NT == SEQ:
            return
        m3 = SB.mark()
        wif = SB.alloc([128, 8, 16], BF16)
        LD(wif[:], w_if[l].rearrange("(k p) n -> p k n", p=128), wif, q="pool")
        bif = SB.alloc([1, 16], F32)
        LD(bif[:], b_if[l], bif)
        vsts = Rot([SB.alloc([128, 512], BF16) for _ in range(3)])
        for blk in range(2):
            wb = load_w(V_OFF + blk * 512)
            for t in range(TT):
                bank = bankrot.next()
                for k in range(8):
                    MM(bank, bank[:, :], uT[:, k, t * 128:(t + 1) * 128], wb[:, k, :], k == 0, k == 7, [uT.b, wb.b])
                vst = vsts.next()
                CP("act" if t % 2 == 0 else "dve", vst[:], bank[:, :], [], [bank.b, vst.b])
                ST(scr["vtm"][t * 128:(t + 1) * 128, blk * 512:(blk + 1) * 512], vst[:], vst, B("vtm", var))
        for t0 in range(0, TT, 16):
            nt = min(16, TT - t0)
            bank = bankrot.next()
            for i in range(nt):
                t = t0 + i
                for k in range(8):
                    MM(bank, bank[:, i * 16:(i + 1) * 16], uT[:, k, t * 128:(t + 1) * 128], wif[:, k, :], k == 0, False,
                       [uT.b, wif.b])
                MM(bank, bank[:, i * 16:(i + 1) * 16], ones[0:1, :], bif[0:1, :], False, True, [cm.b, bif.b])
            CP("dve", gsb[:, t0:t0 + nt, :], bank[:, 0:nt * 16].rearrange("p (t g) -> p t g", g=16), [], [bank.b, gsb.b])
        sgs = Rot([SB.alloc([128, TK], BF16) for _ in range(3)])

        def sig_block(col0, dst, dname):
            for blk in range(2):
                wb = load_w(col0 + blk * 512)
                for c in range(4):
                    cg = blk * 4 + c

                    def ev(bank, tk, cg=cg):
                        sg = sgs.next()
                        ACT(sg[:], bank[:, 0:TK], AF.Sigmoid, [], [bank.b, sg.b])
                        ST(dst[cg * 128:(cg + 1) * 128, tk * TK:(tk + 1) * TK], sg[:], sg, B(dname, var))
                    fm_chunk(wb, c, ev)

        if seq["with_out"]:
            sig_block(O_OFF, scr["oT"], "oT")
            sig_block(GA_OFF, scr["gaT"], "gaT")
            sig_block(GB_OFF, scr["gbT"], "gbT")
        P.barrier()
        SB.release(m3)
        if debug.get("sub") == "A2b" and NT == SEQ:
            return
        if seq["with_out"]:
            m4 = SB.mark()
            wpl = SB.alloc([128, 4, 128], BF16)
            LD(wpl[:], w_pool[l].rearrange("g c e -> c g e"), wpl, q="pool")
            wb = load_w(POOL_OFF)
            pz = SB.alloc([128, NT], F32)
            a1 = SB.alloc([128, NT], F32)
            a2 = SB.alloc([128, NT], F32)
            ivc = SB.alloc([128, NT], F32)
            pmb = SB.alloc([128, NT], BF16)
            sgs = Rot([SB.alloc([128, TK], BF16) for _ in range(3)])
            invc_src = invc_ctx if NT == CTX else invc_lat
            for g in range(4):
                w = POOL_W[g]
                LD(ivc[:], invc_src[g], ivc)
                fm_chunk(wb, g, lambda bank, tk: CP("act", pz[:, tk * TK:(tk + 1) * TK], bank[:, 0:TK], [], [bank.b, pz.b]))
                if NT == CTX:
                    CP("dve", a2[:], pz[:], [pz.b], [a2.b])
                    for o in range(-(w // 2), w // 2):
                        if o == 0:
                            continue
                        lo, hi = max(0, -o), min(NT, NT - o)
                        TT_("dve", a2[:, lo:hi], a2[:, lo:hi], pz[:, lo + o:hi + o], ALU.add, [pz.b], [a2.b])
                else:
                    G = 64
                    pz3 = pz[:].rearrange("p (r x) -> p r x", x=G)
                    a13 = a1[:].rearrange("p (r x) -> p r x", x=G)
                    CP("dve", a1[:], pz[:], [pz.b], [a1.b])
                    for o in range(-(w // 2), w // 2):
                        if o == 0:
                            continue
                        lo, hi = max(0, -o), min(G, G - o)
                        TT_("dve", a13[:, :, lo:hi], a13[:, :, lo:hi], pz3[:, :, lo + o:hi + o], ALU.add, [pz.b], [a1.b])
                    CP("dve", a2[:], a1[:], [a1.b], [a2.b])
                    for o in range(-(w // 2), w // 2):
                        if o == 0:
                            continue
                        lo, hi = max(0, -o), min(G, G - o)
                        TT_("dve", a2[:, lo * G:hi * G], a2[:, lo * G:hi * G], a1[:, (lo + o) * G:(hi + o) * G], ALU.add,
                            [a1.b], [a2.b])
                TT_("dve", a2[:], a2[:], ivc[:], ALU.mult, [ivc.b], [a2.b])
                TT_("dve", pmb[:], a2[:], pz[:], ALU.subtract, [a2.b, pz.b], [pmb.b])
                for tk in range(nTK):
                    bank = bankrot.next()
                    MM(bank, bank[:, 0:TK], wpl[:, g, :], pmb[:, tk * TK:(tk + 1) * TK], True, True, [wpl.b, pmb.b])
                    sg = sgs.next()
                    ACT(sg[:], bank[:, 0:TK], AF.Identity, [lay["vecs"].b], [bank.b, sg.b], scale=lay["vecs"][:, 24 + g:25 + g])
                    ST(scr["pmwT"][g * 128:(g + 1) * 128, tk * TK:(tk + 1) * TK], sg[:], sg, B("pmwT", var))
            P.barrier()
            SB.release(m4)
        P.barrier()
        SB.release(mA)

    def gate_prep(seq):
        TT = seq["NT"] // 128
        gsb = seq["gsb"]
        gd = seq["gd"]
        lf, bc, a_s, wk, ebl = gd["lf"], gd["bc"], gd["a_s"], gd["wk"], gd["ebl"]
        gf = gsb[:, :, 8:16]
        ACT(a_s[:], gf, AF.Abs, [gsb.b], [a_s.b])
        ACT(a_s[:], a_s[:], AF.Exp, [], [a_s.b], scale=-1.0)
        ACT(a_s[:], a_s[:], AF.Ln, [epsc.b], [a_s.b], bias=epsc[:, 1:2])
        TS("dve", lf[:], gf, 0.0, None, ALU.min, None, [gsb.b], [lf.b])
        TT_("dve", lf[:], lf[:], a_s[:], ALU.subtract, [a_s.b], [lf.b])
        for d in range(2):
            bank = bankrot.next()
            for t in range(TT):
                MM(bank, bank[:, t * 4:(t + 1) * 4], tri[d], lf[:, t, d * 4:(d + 1) * 4], True, True, [cm.b, lf.b])
            CP("dve", bc[:, :, d * 4:(d + 1) * 4], bank[:, 0:TT * 4].rearrange("p (t h) -> p t h", h=4), [], [bank.b, bc.b])
        bank = bankrot.next()
        MM(bank, bank[:, 0:TT * 8], ones, lf[:].rearrange("p t g -> p (t g)"), True, True, [cm.b, lf.b])
        CP("dve", ebl[:].rearrange("p t g -> p (t g)"), bank[:, 0:TT * 8], [], [bank.b, ebl.b])
        TT_("dve", a_s[:], gsb[:, :, 0:8], bc[:], ALU.subtract, [gsb.b, bc.b], [a_s.b])
        TT_("dve", wk[:], a_s[:], ebl[:], ALU.add, [a_s.b, ebl.b], [wk.b])
        ACT(wk[:], wk[:], AF.Exp, [epsc.b], [wk.b], bias=epsc[:, 2:3])
        ACT(ebl[:], ebl[:], AF.Exp, [wk.b], [ebl.b])

    def scan_chunk(seq, t, d, data, st, work, with_out, htile):
        gsb, gd = seq["gsb"], seq["gd"]
        lf, bc, wk, ebl = gd["lf"], gd["bc"], gd["wk"], gd["ebl"]
        qTt, kTt, ktmt, vt = data
        Cf, Cb = st
        H = range(NH)
        W = [dict() for _ in H]
        for h in H:
            dh = d * 4 + h
            w = W[h]
            w["dh"] = dh
            w["vw"] = work["vw"].next()
            ACT(w["vw"][:], vt[:, h, :], AF.Identity, [vt.b, wk.b], [w["vw"].b], scale=wk[:, t, dh:dh + 1])
            if with_out:
                for nm in ("lfrep", "arg", "dec", "ebr", "sdT", "qs", "den"):
                    w[nm] = work[nm].next()
                ACT(w["lfrep"][:], ones, AF.Identity, [cm.b, lf.b], [w["lfrep"].b], scale=lf[:, t, dh:dh + 1])
        for h in H:
            W[h]["bx"] = bankrot.next()
        for h in H:
            W[h]["bw"] = bankrot.next()
        for h in H:
            w = W[h]
            dh = w["dh"]
            bx = w["bx"]
            if with_out:
                MM(bx, bx[:, 0:128], w["lfrep"][:], tri[d], True, True, [w["lfrep"].b, cm.b])
                for dc in range(2):
                    MM(bx, bx[:, 128:256], kTt[:, h * 2 + dc, :], qTt[:, h * 2 + dc, :], dc == 0, dc == 1, [kTt.b, qTt.b])
            bw = w["bw"]
            vw = w["vw"]
            for dc in range(2):
                MM(bw, bw[:, dc * 256:(dc + 1) * 256], ktmt[:, h * 256 + dc * 128:h * 256 + (dc + 1) * 128], vw[:, 0:256], True, True,
                   [ktmt.b, vw.b])
            for dc in range(2):
                MM(bx, bx[:, 256 + dc:257 + dc], ktmt[:, h * 256 + dc * 128:h * 256 + (dc + 1) * 128], vw[:, 256:257], True, True,
                   [ktmt.b, vw.b])
        def n_update(h):
            w = W[h]
            dh, bx = w["dh"], w["bx"]
            STT("dve", Cf[:, dh, :, 256:257], Cf[:, dh, :, 256:257], ebl[:, t, dh:dh + 1],
                bx[:, 256:258].rearrange("p (c e) -> p c e", c=2), ALU.mult, ALU.add, [ebl.b], [bx.b, B("Cf", dh)])

        if with_out:
            for h in H:
                w = W[h]
                dh, bx = w["dh"], w["bx"]
                STT("dve", w["arg"][:], bx[:, 0:128], bc[:, t, dh:dh + 1], negm[d], ALU.subtract, ALU.add, [bc.b, cm.b],
                    [bx.b, w["arg"].b])
                ACT(w["ebr"][:], bx[:, 0:128], AF.Exp, [], [bx.b, w["ebr"].b])
            for h in H:
                w = W[h]
                ACT(w["dec"][:], w["arg"][:], AF.Exp, [w["arg"].b, gsb.b], [w["dec"].b], bias=gsb[:, t, w["dh"]:w["dh"] + 1])
            for h in H:
                w = W[h]
                bx = w["bx"]
                STT("dve", w["sdT"][:], bx[:, 128:256], 1.0 / 16.0, w["dec"][:], ALU.mult, ALU.mult, [w["dec"].b], [bx.b, w["sdT"].b])
                n_update(h)
                for dc in range(2):
                    TT_("dve", w["qs"][:, dc, :], qTt[:, h * 2 + dc, :], w["ebr"][:], ALU.mult, [qTt.b, w["ebr"].b], [w["qs"].b])
        for h in H:
            w = W[h]
            dh, bw = w["dh"], w["bw"]
            STT("dve", Cf[:, dh, :, 0:256], Cf[:, dh, :, 0:256], ebl[:, t, dh:dh + 1], bw[:, :].rearrange("p (c e) -> p c e", c=2),
                ALU.mult, ALU.add, [ebl.b], [bw.b, B("Cf", dh)])
        if not with_out:
            for h in H:
                n_update(h)
        if with_out:
            for h in H:
                w = W[h]
                dh = w["dh"]
                bz = w["bz"] = bankrot.next()
                for dc in range(2):
                    MM(bz, bz[:, 0:257], w["qs"][:, dc, :], Cb[:, dh, dc, :], dc == 0, False, [w["qs"].b, B("Cb", dh)])
                MM(bz, bz[:, 0:257], w["sdT"][:], vt[:, h, :], False, True, [w["sdT"].b, vt.b])
        if with_out:
            for h in H:
                w = W[h]
                ACT(w["den"][:, 0:1], w["bz"][:, 256:257], AF.Abs, [], [w["bz"].b, w["den"].b])
        for h in H:
            dh = W[h]["dh"]
            CP("act", Cb[:, dh, :, :], Cf[:, dh, :, :], [B("Cf", dh)], [B("Cb", dh)])
        if with_out:
            for h in H:
                den = W[h]["den"]
                TS("dve", den[:, 0:1], den[:, 0:1], 1.0, None, ALU.max, None, [], [den.b])
            for h in H:
                den = W[h]["den"]
                P.op("dve", lambda e, den=den: e.reciprocal(out=den[:, 0:1], in_=den[:, 0:1]), [], [den.b])
            for h in H:
                w = W[h]
                ACT(htile[:, h * 256:(h + 1) * 256], w["bz"][:, 0:256], AF.Identity, [w["den"].b], [w["bz"].b, htile.b],
                    scale=w["den"][:, 0:1])

    def phase_scan(l, lay, seq, h_src, last_layer):
        NT = seq["NT"]
        TT = NT // 128
        var = seq["var"]
        scr = seq["scr"]
        with_out = seq["with_out"]
        Cf, Cb = seq["Cf"], seq["Cb"]
        gate_prep(seq)
        mB = SB.mark()
        work = {
            "lfrep": Rot([SB.alloc([128, 128], F32) for _ in range(4)]),
            "arg": Rot([SB.alloc([128, 128], F32) for _ in range(4)]),
            "dec": Rot([SB.alloc([128, 128], F32) for _ in range(4)]),
            "ebr": Rot([SB.alloc([128, 128], BF16) for _ in range(4)]),
            "sdT": Rot([SB.alloc([128, 128], BF16) for _ in range(4)]),
            "qs": Rot([SB.alloc([128, 2, 128], BF16) for _ in range(4)]),
            "den": Rot([SB.alloc([128, 2], F32) for _ in range(4)]),
            "vw": Rot([SB.alloc([128, 257], BF16) for _ in range(4)]),
        }
        dq = Rot([SB.alloc([128, 8, 128], BF16) for _ in range(2)])
        dk = Rot([SB.alloc([128, 8, 128], BF16) for _ in range(2)])
        dkt = Rot([SB.alloc([128, D], BF16) for _ in range(2)])
        dv = Rot([SB.alloc([128, 4, 257], BF16) for _ in range(2)])
        for v_ in dv.items:
            MS("dve", v_[:], 1.0, [v_.b])

        def load_chunk(t):
            qTt, kTt, ktmt, vt = dq.next(), dk.next(), dkt.next(), dv.next()
            if with_out:
                LD(qTt[:], scr["qT"][:, t * 128:(t + 1) * 128].rearrange("(c p) t -> p c t", p=128), qTt, reads=[B("qT", var)])
                LD(kTt[:], scr["kT"][:, t * 128:(t + 1) * 128].rearrange("(c p) t -> p c t", p=128), kTt, reads=[B("kT", var)])
            LD(ktmt[:], scr["ktm"][t * 128:(t + 1) * 128, :], ktmt, reads=[B("ktm", var)])
            LD(vt[:, :, 0:256], scr["vtm"][t * 128:(t + 1) * 128, :].rearrange("p (h e) -> p h e", e=256), vt,
               reads=[B("vtm", var)])
            return (qTt, kTt, ktmt, vt)

        hfs = Rot([SB.alloc([128, D], F32) for _ in range(2)])
        nxt = load_chunk(0)
        for t in range(TT):
            data = nxt
            if t + 1 < TT:
                nxt = load_chunk(t + 1)
            hft = hfs.next()
            scan_chunk(seq, t, 0, data, (Cf, Cb), work, with_out, hft)
            if with_out:
                ST(scr["hf"][t * 128:(t + 1) * 128, :], hft[:], hft, B("hf", var))
        if not with_out:
            nxt = load_chunk(TT - 1)
            for t in range(TT - 1, -1, -1):
                data = nxt
                if t > 0:
                    nxt = load_chunk(t - 1)
                scan_chunk(seq, t, 1, data, (Cf, Cb), work, False, None)
            P.barrier()
            SB.release(mB)
            return
        wpa = SB.alloc([128, 8, D], BF16)
        LD(wpa[:], w_pa[l].rearrange("(k p) n -> p k n", p=128), wpa, q="pool")
        wpb = SB.alloc([128, 4, D], BF16)
        LD(wpb[:], w_pb[l].rearrange("(k p) n -> p k n", p=128), wpb, q="pool")
        wo = SB.alloc([128, 8, D], BF16)
        LD(wo[:], w_out[l].rearrange("(k p) n -> p k n", p=128), wo, q="pool")
        GT = min(4, TT)
        GN = GT * 128
        hbs = Rot([SB.alloc([128, D], F32) for _ in range(1)])
        stats = SB.alloc([128, 4, 6], F32)
        mv = SB.alloc([128, 4, 2], F32)
        hmT = SB.alloc([128, 8, GN], BF16)
        oTt = SB.alloc([128, 8, GN], BF16)
        gaTt = SB.alloc([128, 8, GN], BF16)
        gbTt = SB.alloc([128, 8, GN], BF16)
        pmTt = SB.alloc([128, 4, GN], BF16)
        mT = SB.alloc([128, 8, GN], BF16)
        t1s = Rot([SB.alloc([128, GN], F32) for _ in range(2)])
        t2s = Rot([SB.alloc([128, GN], F32) for _ in range(1)])
        hos = Rot([SB.alloc([128, D], F32) for _ in range(1)])
        hns = Rot([SB.alloc([128, D], F32) for _ in range(2)])
        tmps = Rot([(SB.alloc([128, 2], F32), SB.alloc([128, D], F32)) for _ in range(2)])
        u2s = Rot([SB.alloc([128, 8, 128], BF16) for _ in range(2)])
        u2f = SB.alloc([128, 8, 128], F32) if last_layer else None
        if last_layer:
            wr = SB.alloc([128, 8, NE], F32)
            LD(wr[:], w_router[0].rearrange("(k p) n -> p k n", p=128), wr)
            mx = SB.alloc([128, 8], F32)
            lg = SB.alloc([128, 8], F32)
            msk = SB.alloc([128, 8], F32)
            sm = SB.alloc([128, 4], F32)
        gt1 = lay["gtb"]["gt1" + ("x" if var == 0 else "c")]
        vecs = lay["vecs"]
        def load_chunk_b(t):
            dat = load_chunk(t)
            hf_ = hfs.next()
            LD(hf_[:], scr["hf"][t * 128:(t + 1) * 128, :], hf_, reads=[B("hf", var)])
            return dat, hf_

        def tail_tile(g, ti):
            t = g * GT + ti
            ho, hn = hos.next(), hns.next()
            LD(ho[:], h_src[t * 128:(t + 1) * 128, :], ho, reads=[seq["hbuf"]])
            for hf in range(2):
                bank = bankrot.next()
                for k in range(8):
                    MM(bank, bank[:, :], mT[:, k, ti * 128:(ti + 1) * 128], wo[:, k, hf * 512:(hf + 1) * 512], k == 0, k == 7,
                       [mT.b, wo.b])
                TT_("dve", hn[:, hf * 512:(hf + 1) * 512], bank[:, :], gt1[:, hf * 512:(hf + 1) * 512], ALU.mult, [gt1.b],
                    [bank.b, hn.b])
            TT_("dve", hn[:], hn[:], ho[:], ALU.add, [ho.b], [hn.b])
            ST(scr["h1"][t * 128:(t + 1) * 128, :], hn[:], hn, B("h1", var), q="pool")
            u2 = u2s.next()
            if last_layer:
                norm_transpose(hn[:], [hn.b], lambda k: (lay["s2"][:, k, var:var + 1], lay["s2"].b),
                               lambda k: (lay["modT"][:, 24 + k, var:var + 1], lay["modT"].b),
                               lambda k: (u2f[:, k, :], [u2f.b]), tmps.next())
                CP("act", u2[:], u2f[:], [u2f.b], [u2.b])
                bank = bankrot.next()
                for k in range(8):
                    MM(bank, bank[:, 0:NE], u2f[:, k, :], wr[:, k, :], k == 0, k == 7, [u2f.b, wr.b])
                CP("dve", lg[:], bank[:, 0:NE], [], [bank.b, lg.b])
                P.op("dve", lambda e: e.max(out=mx[:], in_=lg[:]), [lg.b], [mx.b])
                TS("dve", msk[:], lg[:], mx[:, 1:2], None, ALU.is_ge, None, [lg.b, mx.b], [msk.b])
                TS("dve", sm[:, 0:1], mx[:, 0:1], -1.0, None, ALU.mult, None, [mx.b], [sm.b])
                ACT(lg[:], lg[:], AF.Exp, [sm.b], [lg.b], bias=sm[:, 0:1])
                ACT(sm[:, 1:2], mx[:, 1:2], AF.Exp, [sm.b, mx.b], [sm.b], bias=sm[:, 0:1])
                TS("dve", sm[:, 1:2], sm[:, 1:2], 1.0, None, ALU.add, None, [], [sm.b])
                P.op("dve", lambda e: e.reciprocal(out=sm[:, 1:2], in_=sm[:, 1:2]), [], [sm.b])
                TT_("dve", lg[:], lg[:], msk[:], ALU.mult, [msk.b], [lg.b])
                TS("dve", seq["moeg"][:, t, :], lg[:], sm[:, 1:2], None, ALU.mult, None, [lg.b, sm.b], [seq["moeg"].b])
            else:
                norm_transpose(hn[:], [hn.b], lambda k: (lay["s2"][:, k, var:var + 1], lay["s2"].b),
                               lambda k: (lay["modT"][:, 24 + k, var:var + 1], lay["modT"].b),
                               lambda k: (u2[:, k, :], [u2.b]), tmps.next())
            ST(scr["u2T"][:, t * 128:(t + 1) * 128].rearrange("(c p) t -> p c t", p=128), u2[:], u2, B("u2T", var), q="pool")

        nxt = load_chunk_b(TT - 1)
        pending = []
        for g in range(TT // GT - 1, -1, -1):
            T0 = g * GN
            LD(oTt[:], scr["oT"][:, T0:T0 + GN].rearrange("(c p) t -> p c t", p=128), oTt, q="pool", reads=[B("oT", var)])
            LD(gaTt[:], scr["gaT"][:, T0:T0 + GN].rearrange("(c p) t -> p c t", p=128), gaTt, q="pool", reads=[B("gaT", var)])
            LD(gbTt[:], scr["gbT"][:, T0:T0 + GN].rearrange("(c p) t -> p c t", p=128), gbTt, q="pool", reads=[B("gbT", var)])
            LD(pmTt[:], scr["pmwT"][:, T0:T0 + GN].rearrange("(c p) t -> p c t", p=128), pmTt, q="pool", reads=[B("pmwT", var)])
            for ti in range(GT - 1, -1, -1):
                t = g * GT + ti
                data, hft = nxt
                if t > 0:
                    nxt = load_chunk_b(t - 1)
                hbt = hbs.next()
                scan_chunk(seq, t, 1, data, (Cf, Cb), work, True, hbt)
                TT_("dve", hbt[:], hbt[:], hft[:], ALU.add, [hft.b], [hbt.b])
                for h in range(NH):
                    P.op("dve", lambda e, h=h, hbt=hbt: e.bn_stats(out=stats[:, h, :], in_=hbt[:, h * 256:(h + 1) * 256]),
                         [hbt.b], [stats.b])
                    P.op("dve", lambda e, h=h: e.bn_aggr(out=mv[:, h, :], in_=stats[:, h, :]), [stats.b], [mv.b])
                ACT(mv[:, :, 1], mv[:, :, 1], AF.Sqrt, [epsc.b], [mv.b], bias=epsc[:, 0:1])
                P.op("dve", lambda e: e.reciprocal(out=mv[:, :, 1], in_=mv[:, :, 1]), [], [mv.b])
                for h in range(NH):
                    TS("dve", hbt[:, h * 256:(h + 1) * 256], hbt[:, h * 256:(h + 1) * 256], mv[:, h, 0:1], mv[:, h, 1:2],
                       ALU.subtract, ALU.mult, [mv.b], [hbt.b])
                for hf in range(2):
                    bank = bankrot.next()
                    for i in range(4):
                        k = hf * 4 + i
                        TR(bank, bank[:, i * 128:(i + 1) * 128], hbt[:, k * 128:(k + 1) * 128], ident, [hbt.b, cm.b])
                    for i in range(4):
                        k = hf * 4 + i
                        STT("dve", hmT[:, k, ti * 128:(ti + 1) * 128], bank[:, i * 128:(i + 1) * 128], vecs[:, 8 + k:9 + k],
                            oTt[:, k, ti * 128:(ti + 1) * 128], ALU.mult, ALU.mult, [vecs.b, oTt.b], [bank.b, hmT.b])
                if pending:
                    tail_tile(*pending.pop(0))
            while pending:
                tail_tile(*pending.pop(0))
            for f in range(8):
                ba = bankrot.next()
                for k in range(8):
                    MM(ba, ba[:, 0:GN], wpa[:, k, f * 128:(f + 1) * 128], hmT[:, k, :], k == 0, k == 7, [wpa.b, hmT.b])
                bb = bankrot.next()
                for k in range(4):
                    MM(bb, bb[:, 0:GN], wpb[:, k, f * 128:(f + 1) * 128], pmTt[:, k, :], k == 0, k == 3, [wpb.b, pmTt.b])
                t1, t2 = t1s.next(), t2s.next()
                TT_("dve", t1[:], ba[:, 0:GN], gaTt[:, f, :], ALU.mult, [gaTt.b], [ba.b, t1.b])
                TT_("dve", t2[:], bb[:, 0:GN], gbTt[:, f, :], ALU.mult, [gbTt.b], [bb.b, t2.b])
                TT_("dve", mT[:, f, :], t1[:], t2[:], ALU.add, [t1.b, t2.b], [mT.b])
            pending = [(g, ti) for ti in range(GT)]
        while pending:
            tail_tile(*pending.pop(0))
        seq["scan_peak"] = SB.lo
        P.barrier()
        SB.release(mB)

    def phase_ffn(l, lay, seq, experts, nff, h_dst, final):
        NT = seq["NT"]
        var = seq["var"]
        scr = seq["scr"]
        TB = min(1024, NT)
        nTB = NT // TB
        SUBW = min(512, TB)
        nsub = TB // SUBW
        nts = TB // 128
        NJ = nff // 128
        mF = SB.mark()
        u2 = SB.alloc([128, 8, TB], BF16)
        act = SB.alloc([128, NJ, TB], BF16)
        yacc = SB.alloc([128, nts, D], F32)
        wds = Rot([SB.alloc([128, NJ, 512], BF16) for _ in range(2)])
        WB = 128
        wgs = Rot([SB.alloc([128, 8, WB], BF16) for _ in range(2)])
        wus = Rot([SB.alloc([128, 8, WB], BF16) for _ in range(2)])
        sgs = Rot([SB.alloc([128, SUBW], BF16) for _ in range(2)])
        hos = Rot([SB.alloc([128, D], F32) for _ in range(2)])
        gt2 = lay["gtb"]["gt2" + ("x" if var == 0 else "c")]
        if final:
            gfin = SB.alloc([128, D], F32)
            LD(gfin[:], gfin_in, gfin)
            tmpf = Rot([(SB.alloc([128, 2], F32), SB.alloc([128, D], F32)) for _ in range(1)])
        moeg = seq.get("moeg")
        def residual_tile(T0, ts):
            t = T0 // 128 + ts
            ho = hos.next()
            LD(ho[:], scr["h1"][t * 128:(t + 1) * 128, :], ho, reads=[B("h1", var)])
            ybs = [B("yacc", ts, 0), B("yacc", ts, 1)]
            TT_("dve", yacc[:, ts, :], yacc[:, ts, :], gt2[:], ALU.mult, [gt2.b], ybs)
            TT_("dve", ho[:], ho[:], yacc[:, ts, :], ALU.add, ybs, [ho.b])
            if not final:
                ST(h_dst[0][t * 128:(t + 1) * 128, :], ho[:], ho, h_dst[1])
            else:
                ss, xn = tmpf.next()
                ACT(xn[:], ho[:], AF.Square, [ho.b], [xn.b, ss.b], accum_out=ss[:, 0:1])
                ACT(ss[:, 1:2], ss[:, 0:1], AF.Sqrt, [epsc.b], [ss.b], scale=1.0 / D, bias=epsc[:, 0:1])
                P.op("dve", lambda e, ss=ss: e.reciprocal(out=ss[:, 1:2], in_=ss[:, 1:2]), [], [ss.b])
                STT("dve", xn[:], ho[:], ss[:, 1:2], gfin[:], ALU.mult, ALU.mult, [ho.b, ss.b, gfin.b], [xn.b])
                ST(y_out[t * 128:(t + 1) * 128, :], xn[:], xn, B("y"))

        def load_u2(tb_):
            LD(u2[:], scr["u2T"][:, tb_ * TB:(tb_ + 1) * TB].rearrange("(c p) t -> p c t", p=128), u2, reads=[B("u2T", var)])

        load_u2(0)
        pend_res = []
        for tb in range(nTB):
            T0 = tb * TB
            for ei, (wg_ap, wu_ap, wd_ap) in enumerate(experts):
                wd0 = None
                for j0 in range(0, nff, WB):
                    if j0 == 2 * WB:
                        wd0 = wds.next()
                        LD(wd0[:], wd_ap[:, 0:512].rearrange("(j p) n -> p j n", p=128), wd0, q="pool")
                    ncol = min(WB, nff - j0)
                    wg, wu = wgs.next(), wus.next()
                    LD(wg[:, :, 0:ncol], wg_ap[:, j0:j0 + ncol].rearrange("(k p) n -> p k n", p=128), wg, q="pool")
                    LD(wu[:, :, 0:ncol], wu_ap[:, j0:j0 + ncol].rearrange("(k p) n -> p k n", p=128), wu, q="pool")
                    for c in range(ncol // 128):
                        j = j0 // 128 + c
                        for s in range(nsub):
                            bg = bankrot.next()
                            for k in range(8):
                                MM(bg, bg[:, 0:SUBW], wg[:, k, c * 128:(c + 1) * 128], u2[:, k, s * SUBW:(s + 1) * SUBW], k == 0,
                                   k == 7, [wg.b, u2.b])
                            bu = bankrot.next()
                            for k in range(8):
                                MM(bu, bu[:, 0:SUBW], wu[:, k, c * 128:(c + 1) * 128], u2[:, k, s * SUBW:(s + 1) * SUBW], k == 0,
                                   k == 7, [wu.b, u2.b])
                            sg = sgs.next()
                            ACT(sg[:], bg[:, 0:SUBW], AF.Silu, [], [bg.b, sg.b])
                            TT_("dve", act[:, j, s * SUBW:(s + 1) * SUBW], bu[:, 0:SUBW], sg[:], ALU.mult, [sg.b], [bu.b, act.b])
                    if pend_res and (j0 // WB) % 2 == 1:
                        residual_tile(*pend_res.pop(0))
                while pend_res:
                    residual_tile(*pend_res.pop(0))
                if ei == len(experts) - 1 and tb + 1 < nTB:
                    load_u2(tb + 1)
                for hf in range(2):
                    if hf == 0:
                        wd = wd0
                    else:
                        wd = wds.next()
                        LD(wd[:], wd_ap[:, hf * 512:(hf + 1) * 512].rearrange("(j p) n -> p j n", p=128), wd, q="pool")
                    for ts in range(nts):
                        bank = bankrot.next()
                        for j in range(NJ):
                            MM(bank, bank[:, :], act[:, j, ts * 128:(ts + 1) * 128], wd[:, j, :], j == 0, j == NJ - 1, [act.b, wd.b])
                        ya = yacc[:, ts, hf * 512:(hf + 1) * 512]
                        yb = B("yacc", ts, hf)
                        if moeg is None:
                            CP("act", ya, bank[:, :], [], [bank.b, yb])
                        else:
                            tg = (T0 // 128) + ts
                            if ei == 0:
                                ACT(ya, bank[:, :], AF.Identity, [moeg.b], [bank.b, yb], scale=moeg[:, tg, ei:ei + 1])
                            else:
                                STT("dve", ya, bank[:, :], moeg[:, tg, ei:ei + 1], ya, ALU.mult, ALU.add, [moeg.b], [bank.b, yb])
            pend_res = [(T0, ts) for ts in range(nts)]
        while pend_res:
            residual_tile(*pend_res.pop(0))
        P.barrier()
        SB.release(mF)

    stop = debug.get("stop")
    hx_src, hx_buf = x_in, B("h_in_x")
    hc_src, hc_buf = ctx_in, B("h_in_c")
    for l in range(2):
        last = l == 1
        mL = SB.mark()
        lay = adaln(l, want_ctx_gt=not last)
        moeg_t = SB.alloc([128, SEQ // 128, NE], F32) if last else None
        mM = SB.mark()
        Cf = SB.alloc([128, 8, 2, 257], F32)
        Cb = SB.alloc([128, 8, 2, 257], BF16)
        MS("dve", Cf[:], 0.0, [B("Cf", i) for i in range(8)])
        MS("dve", Cb[:], 0.0, [B("Cb", i) for i in range(8)])

        def mkseq(NT, var, scr, with_out, hbuf):
            TT = NT // 128
            s = dict(NT=NT, var=var, scr=scr, with_out=with_out, hbuf=hbuf, Cf=Cf, Cb=Cb)
            s["gsb"] = SB.alloc([128, TT, 16], F32)
            s["gd"] = {n: SB.alloc([128, TT, 8], F32) for n in ("lf", "bc", "a_s", "wk", "ebl")}
            return s

        sc = mkseq(CTX, 1, scr_c, not last, hc_buf)
        sx = mkseq(SEQ, 0, scr_x, True, hx_buf)
        if last:
            sx["moeg"] = moeg_t
        if not debug.get("skip_ctx"):
            phase_proj(l, lay, sc, hc_src)
            phase_scan(l, lay, sc, hc_src, False)
        if stop == ("ctx_scan", l):
            break
        phase_proj(l, lay, sx, hx_src)
        if stop == ("proj", l):
            break
        phase_scan(l, lay, sx, hx_src, last)
        if stop == ("scan", l):
            break
        P.barrier()
        SB.release(mM)
        if not last:
            dense = [(w_ffg[0], w_ffu[0], w_ffd[0])]
            phase_ffn(l, lay, sx, dense, DFF, (scr_x["h2"], B("h2", 0)), False)
            phase_ffn(l, lay, sc, dense, DFF, (scr_c["h2"], B("h2", 1)), False)
            hx_src, hx_buf = scr_x["h2"], B("h2", 0)
            hc_src, hc_buf = scr_c["h2"], B("h2", 1)
        else:
            experts = [(w_eg[0, e], w_eu[0, e], w_ed[0, e]) for e in range(NE)]
            phase_ffn(l, lay, sx, experts, DFE, None, True)
        P.barrier()
        SB.release(mL)
    stats = P.finalize()
    return nc, stats


def _consts():
    i = np.arange(128)
    ident = np.eye(128, dtype=np.float32)
    triF = (i[:, None] <= i[None, :]).astype(np.float32)
    triB = (i[:, None] >= i[None, :]).astype(np.float32)
    negF = np.where(i[:, None] <= i[None, :], 0.0, NEG).astype(np.float32)
    negB = np.where(i[:, None] >= i[None, :], 0.0, NEG).astype(np.float32)
    ones = np.ones((128, 128), np.float32)
    cmat = np.stack([ident, triF, triB, negF, negB, ones]).astype(np.float32)

    def cnt(n, w):
        t = np.arange(n)
        lo = np.clip(t - w // 2, 0, n)
        hi = np.clip(t + w // 2, 0, n)
        return (hi - lo).astype(np.float32)

    lat = []
    cx = []
    for w in POOL_W:
        c1 = cnt(64, w)
        lat.append(np.broadcast_to((1.0 / (c1[:, None] * c1[None, :])).reshape(1, SEQ), (128, SEQ)))
        cx.append(np.broadcast_to((1.0 / cnt(CTX, w)).reshape(1, CTX), (128, CTX)))
    return cmat, np.ascontiguousarray(np.stack(lat), np.float32), np.ascontiguousarray(np.stack(cx), np.float32)


def _pk(v, k):
    return np.ascontiguousarray(np.asarray(v, np.float32).reshape(k, 128).T)


def prep_inputs(inputs):
    f = lambda a: np.ascontiguousarray(np.asarray(a, dtype=np.float32))
    x, c, ctx, c_ctx = f(inputs["x"]), f(inputs["c"]), f(inputs["ctx"]), f(inputs["c_ctx"])
    w_in = f(inputs["w_in"])
    cmat, invc_lat, invc_ctx = _consts()
    perm = [d * 8 + g * 4 + h for g in range(2) for d in range(2) for h in range(4)]
    w_if = np.ascontiguousarray(w_in[:, :, IF_OFF:IF_OFF + 16][:, :, perm])
    b_if = np.ascontiguousarray(f(inputs["b_if"]).transpose(0, 2, 1, 3).reshape(2, 1, 16))
    vecs = np.stack([np.concatenate([_pk(inputs["g_mix"][l], 8), _pk(inputs["head_gain"][l], 8), _pk(inputs["g_ffn"][l], 8),
                                     _pk(inputs["pool_scale"][l], 4)], axis=1) for l in range(2)])
    convw = np.stack([np.ascontiguousarray(f(inputs["conv_qk"])[l].reshape(3, 16, 128).transpose(2, 1, 0)) for l in range(2)])
    shared = {
        "w_ada": f(inputs["w_ada"]), "b_ada": f(inputs["b_ada"]).reshape(2, 1, 6 * D),
        "vecs": np.ascontiguousarray(vecs, np.float32), "convw": np.ascontiguousarray(convw, np.float32),
        "w_in": w_in, "w_if": w_if, "b_if": b_if,
        "w_pool": f(inputs["w_pool"]), "w_pa": f(inputs["w_pa"]), "w_pb": f(inputs["w_pb"]), "w_out": f(inputs["w_out"]),
        "w_ff_gate": f(inputs["w_ff_gate"]), "w_ff_up": f(inputs["w_ff_up"]), "w_ff_down": f(inputs["w_ff_down"]),
        "w_router": f(inputs["w_router"]), "w_exp_gate": f(inputs["w_exp_gate"]), "w_exp_up": f(inputs["w_exp_up"]),
        "w_exp_down": f(inputs["w_exp_down"]),
        "g_final_b": np.ascontiguousarray(np.broadcast_to(f(inputs["g_final"])[None, :], (128, D))),
        "cmat": cmat, "invc_lat": invc_lat, "invc_ctx": invc_ctx,
    }
    in_maps = []
    for b in range(8):
        m = dict(shared)
        m["x"] = x[b]
        m["ctx"] = ctx[b]
        m["cc"] = np.ascontiguousarray(np.stack([_pk(c[b], 8), _pk(c_ctx, 8)], axis=-1))
        in_maps.append(m)
    return in_maps


def kernel(**inputs):
    in_maps = prep_inputs(inputs)
    nc, _ = build()
    res = run_bass_kernel_spmd(nc, in_maps, core_ids=list(range(8)))
    return np.stack([np.asarray(r["y"], dtype=np.float32) for r in res.results], axis=0)
```

```python
import numpy as np
import concourse.bass as bass
import concourse.mybir as mybir
from concourse.bass_utils import run_bass_kernel_spmd

F32 = mybir.dt.float32
BF16 = mybir.dt.bfloat16
AF = mybir.ActivationFunctionType
ALU = mybir.AluOpType

EPOCH = 20000
N_DMA_TL = 12


class Buf:
    __slots__ = ("name", "last_writer", "readers", "readers_d")

    def __init__(self, name):
        self.name = name
        self.last_writer = None
        self.readers = {}
        self.readers_d = []


class Op:
    __slots__ = ("eng", "fn", "reads", "writes", "dma", "deps", "signal", "sig", "id", "barrier")


class Prog:
    ENGS = ("pe", "act", "dve", "pool", "sp")

    def __init__(self, nc):
        self.nc = nc
        self.ops = []
        self.bufs = {}
        self._uid = 0

    def buf(self, *key):
        b = self.bufs.get(key)
        if b is None:
            b = Buf(key)
            self.bufs[key] = b
        return b

    def newbuf(self, tag="b"):
        self._uid += 1
        return self.buf(tag, self._uid)

    def op(self, eng, fn, reads=(), writes=(), dma=False):
        o = Op()
        o.eng = eng
        o.fn = fn
        o.reads = tuple(reads)
        o.writes = tuple(writes)
        o.dma = dma
        o.id = len(self.ops)
        o.signal = dma
        o.sig = None
        o.barrier = False
        o.deps = ()
        self.ops.append(o)
        return o

    def barrier(self):
        o = self.op(None, None)
        o.barrier = True
        return o

    def dma(self, eng, out, in_, reads=(), writes=(), **kw):
        return self.op(eng, lambda e: e.dma_start(out=out, in_=in_, **kw), reads, writes, dma=True)

    def finalize(self):
        nc = self.nc
        ops = self.ops
        last_on = {e: None for e in self.ENGS}
        for o in ops:
            if o.barrier:
                for e in self.ENGS:
                    if last_on[e] is not None:
                        ops[last_on[e]].signal = True
                for b in self.bufs.values():
                    b.last_writer = None
                    b.readers = {}
                    b.readers_d = []
                continue
            deps = set()
            for b in o.reads:
                if b.last_writer is not None:
                    deps.add(b.last_writer)
            for b in o.writes:
                if b.last_writer is not None:
                    deps.add(b.last_writer)
                deps.update(b.readers.values())
                deps.update(b.readers_d)
            for b in o.writes:
                b.last_writer = o.id
                b.readers = {}
                b.readers_d = []
            for b in o.reads:
                if b.last_writer != o.id:
                    if o.dma:
                        b.readers_d.append(o.id)
                    else:
                        b.readers[o.eng] = o.id
            deps.discard(o.id)
            fd = []
            for d in deps:
                p = ops[d]
                if (not p.dma) and (not o.dma) and p.eng == o.eng and o.eng == "pe":
                    continue
                fd.append(d)
            o.deps = fd
            for d in fd:
                ops[d].signal = True
            if not o.dma:
                last_on[o.eng] = o.id
        cnt = {e: 0 for e in self.ENGS}
        dma_rr = {e: 0 for e in self.ENGS}
        dma_uses = {}
        for o in ops:
            if o.barrier:
                continue
            if o.dma:
                k = dma_rr[o.eng] % N_DMA_TL
                dma_rr[o.eng] += 1
                tl = ("dma", o.eng, k)
                dma_uses[tl] = dma_uses.get(tl, 0) + 1
                o.sig = (tl, dma_uses[tl])
            elif o.signal:
                cnt[o.eng] += 1
                o.sig = (("eng", o.eng), cnt[o.eng])
        sems = {}
        for e in self.ENGS:
            n_ep = (cnt[e] + EPOCH - 1) // EPOCH
            for ep in range(max(n_ep, 1)):
                sems[(("eng", e), ep)] = nc.alloc_semaphore(f"s_{e}_{ep}")
        for tl in dma_uses:
            sems[(tl, 0)] = nc.alloc_semaphore(f"s_dma_{tl[1]}_{tl[2]}")

        def sem_of(tl, v):
            if tl[0] == "eng":
                ep = (v - 1) // EPOCH
                return sems[(tl, ep)], v - ep * EPOCH
            return sems[(tl, 0)], v * 16

        seen = {e: {} for e in self.ENGS}
        pending = {e: None for e in self.ENGS}
        cur = {}
        streams = {e: [] for e in self.ENGS}
        n_waits = 0
        for o in ops:
            if o.barrier:
                snap = dict(cur)
                for e in self.ENGS:
                    if pending[e] is None:
                        pending[e] = dict(snap)
                    else:
                        pending[e].update(snap)
                continue
            E = o.eng
            need = {}
            if pending[E] is not None:
                need.update(pending[E])
                pending[E] = None
            for d in o.deps:
                tl, v = ops[d].sig
                if need.get(tl, 0) < v:
                    need[tl] = v
            if o.dma:
                tl, v = o.sig
                if v > 1 and need.get(tl, 0) < v - 1:
                    need[tl] = v - 1
            waits = []
            for tl, v in need.items():
                if seen[E].get(tl, 0) < v:
                    seen[E][tl] = v
                    waits.append(sem_of(tl, v))
            n_waits += len(waits)
            inc = None
            if o.sig is not None:
                tl, v = o.sig
                cur[tl] = v
                s, _ = sem_of(tl, v)
                inc = (s, 16 if tl[0] == "dma" else 1)
            streams[E].append((waits, o.fn, inc))
        fin = []
        for tl, v in cur.items():
            fin.append(sem_of(tl, v))
        self.stats = dict(n_ops=len(ops), n_waits=n_waits, cnt=dict(cnt), n_dma=sum(dma_uses.values()),
                          n_sems=len(sems))
        engmap = {"pe": "tensor", "act": "scalar", "dve": "vector", "pool": "gpsimd", "sp": "sync"}
        with nc.Block() as block:
            for e in self.ENGS:
                lst = streams[e]
                is_sp = e == "sp"

                def body(eng, lst=lst, is_sp=is_sp):
                    for waits, fn, inc in lst:
                        for s, v in waits:
                            eng.wait_ge(s, v)
                        ins = fn(eng)
                        if inc is not None:
                            ins.then_inc(inc[0], inc[1])
                    if is_sp:
                        for s, v in fin:
                            eng.wait_ge(s, v)

                if lst or is_sp:
                    getattr(block, engmap[e])(body)
        return self.stats


class T:
    __slots__ = ("t", "b")

    def __init__(self, t, b):
        self.t = t
        self.b = b

    def __getitem__(self, k):
        return self.t[k]


class SBA:
    BASE = 16512
    END = 229376

    def __init__(self, nc, P):
        self.nc = nc
        self.P = P
        self.lo = self.BASE
        self.n = 0

    def alloc(self, shape, dtype):
        sz = 4 if dtype == F32 else 2
        n = sz
        for s in shape[1:]:
            n *= s
        n = (n + 63) // 64 * 64
        off = self.lo
        self.lo += n
        assert self.lo <= self.END, f"SBUF overflow {self.lo}"
        self.peak = max(getattr(self, "peak", 0), self.lo)
        self.n += 1
        t = self.nc.alloc_sbuf_tensor_at(f"sb{self.n}", list(shape), dtype, offset=off)
        return T(t, self.P.newbuf("sb"))

    def mark(self):
        return self.lo

    def release(self, m):
        self.lo = m


class Rot:
    def __init__(self, items):
        self.items = items
        self.i = 0

    def next(self):
        it = self.items[self.i % len(self.items)]
        self.i += 1
        return it


D = 1024
SEQ = 4096
CTX = 256
NH = 4
DH = 256
DFF = 2816
NE = 8
DFE = 3584
EPS = 1e-6
IN_COLS = 6672
POOL_W = (2, 4, 8, 16)
Q_OFF, K_OFF, V_OFF, O_OFF, IF_OFF = 0, 1024, 2048, 3072, 4096
POOL_OFF = 4112
GA_OFF = POOL_OFF + 512
GB_OFF = GA_OFF + 1024
NEG = -30000.0


def build(debug=None):
    nc = bass.Bass("TRN2", target_bir_lowering=False)
    P = Prog(nc)
    SB = SBA(nc, P)

    def din(name, shape, dt=F32):
        return nc.dram_tensor(name, list(shape), dt, kind="ExternalInput").ap()

    debug = debug or {}
    dbg_names = set(debug.get("names", ()))

    def dscr(name, shape, dt):
        kind = "ExternalOutput" if name in dbg_names else "Internal"
        return nc.dram_tensor(name, list(shape), dt, kind=kind).ap()

    x_in = din("x", [SEQ, D])
    ctx_in = din("ctx", [CTX, D])
    cc_in = din("cc", [128, 8, 2])
    w_ada = din("w_ada", [2, D, 6 * D])
    b_ada = din("b_ada", [2, 1, 6 * D])
    vecs_in = din("vecs", [2, 128, 28])
    convw_in = din("convw", [2, 128, 16, 3])
    w_in = din("w_in", [2, D, IN_COLS])
    w_if = din("w_if", [2, D, 16])
    b_if = din("b_if", [2, 1, 16])
    w_pool = din("w_pool", [2, 4, 128, 128])
    w_pa = din("w_pa", [2, D, D])
    w_pb = din("w_pb", [2, 512, D])
    w_out = din("w_out", [2, D, D])
    w_ffg = din("w_ff_gate", [1, D, DFF])
    w_ffu = din("w_ff_up", [1, D, DFF])
    w_ffd = din("w_ff_down", [1, DFF, D])
    w_router = din("w_router", [1, D, NE])
    w_eg = din("w_exp_gate", [1, NE, D, DFE])
    w_eu = din("w_exp_up", [1, NE, D, DFE])
    w_ed = din("w_exp_down", [1, NE, DFE, D])
    gfin_in = din("g_final_b", [128, D])
    cmat_in = din("cmat", [6, 128, 128])
    invc_lat = din("invc_lat", [4, 128, SEQ])
    invc_ctx = din("invc_ctx", [4, 128, CTX])
    y_out = nc.dram_tensor("y", [SEQ, D], F32, kind="ExternalOutput").ap()

    def mk_scr(sfx, NT):
        s = {}
        s["qT"] = dscr("qT" + sfx, [D, NT], BF16)
        s["kT"] = dscr("kT" + sfx, [D, NT], BF16)
        s["ktm"] = dscr("ktm" + sfx, [NT, D], BF16)
        s["vtm"] = dscr("vtm" + sfx, [NT, D], BF16)
        s["oT"] = dscr("oT" + sfx, [D, NT], BF16)
        s["gaT"] = dscr("gaT" + sfx, [D, NT], BF16)
        s["gbT"] = dscr("gbT" + sfx, [D, NT], BF16)
        s["pmwT"] = dscr("pmwT" + sfx, [512, NT], BF16)
        s["hf"] = dscr("hf" + sfx, [NT, D], F32)
        s["u2T"] = dscr("u2T" + sfx, [D, NT], BF16)
        s["h1"] = dscr("h1" + sfx, [NT, D], F32)
        s["h2"] = dscr("h2" + sfx, [NT, D], F32)
        return s

    scr_x = mk_scr("_x", SEQ)
    scr_c = mk_scr("_c", CTX)
    B = P.buf

    banks = []
    for i in range(8):
        banks.append(T(nc.alloc_psum_tensor(f"ps{i}", [128, 512], F32), B("bank", i)))
    bankrot = Rot(banks)

    def rb(*ts):
        return [t.b for t in ts]

    def ACT(out, in_, func, reads, writes, **kw):
        P.op("act", lambda e: e.activation(out=out, in_=in_, func=func, **kw), reads, writes)

    def TS(eng, out, in0, s1, s2, op0, op1, reads, writes):
        if op1 is None:
            P.op(eng, lambda e: e.tensor_scalar(out=out, in0=in0, scalar1=s1, scalar2=None, op0=op0), reads, writes)
        else:
            P.op(eng, lambda e: e.tensor_scalar(out=out, in0=in0, scalar1=s1, scalar2=s2, op0=op0, op1=op1), reads, writes)

    def TT_(eng, out, in0, in1, op, reads, writes):
        P.op(eng, lambda e: e.tensor_tensor(out=out, in0=in0, in1=in1, op=op), reads, writes)

    def STT(eng, out, in0, scalar, in1, op0, op1, reads, writes):
        P.op(eng, lambda e: e.scalar_tensor_tensor(out=out, in0=in0, scalar=scalar, in1=in1, op0=op0, op1=op1), reads, writes)

    def CP(eng, out, in_, reads, writes):
        if eng == "act":
            P.op(eng, lambda e: e.activation(out=out, in_=in_, func=AF.Identity), reads, writes)
        else:
            P.op(eng, lambda e: e.tensor_copy(out=out, in_=in_), reads, writes)

    def MS(eng, ap, val, writes):
        P.op(eng, lambda e: e.memset(ap, val), (), writes)

    def MM(bank, out, lhsT, rhs, start, stop, reads):
        P.op("pe", lambda e: e.matmul(out, lhsT=lhsT, rhs=rhs, start=start, stop=stop), reads, [bank.b])

    def TR(bank, out, in_, ident, reads):
        P.op("pe", lambda e: e.transpose(out=out, in_=in_, identity=ident), reads, [bank.b])

    def LD(dst_ap, src_ap, tile, q="sp", reads=(), **kw):
        P.dma(q, dst_ap, src_ap, reads=reads, writes=[tile.b], **kw)

    def ST(dst_ap, src_ap, tile, dbuf, q="sp"):
        P.dma(q, dst_ap, src_ap, reads=[tile.b], writes=[dbuf])

    cm = SB.alloc([128, 6, 128], F32)
    LD(cm[:], cmat_in.rearrange("c p n -> p c n"), cm)
    ident = cm[:, 0, :]
    tri = (cm[:, 1, :], cm[:, 2, :])
    negm = (cm[:, 3, :], cm[:, 4, :])
    ones = cm[:, 5, :]
    epsc = SB.alloc([128, 4], F32)
    MS("dve", epsc[:, 0:1], EPS, [epsc.b])
    MS("dve", epsc[:, 1:2], 1.0, [epsc.b])
    MS("dve", epsc[:, 2:3], float(-np.log(16.0)), [epsc.b])
    MS("dve", epsc[:, 3:4], 0.0, [epsc.b])
    scc = SB.alloc([128, 8, 2], F32)
    LD(scc[:], cc_in, scc)
    ACT(scc[:], scc[:], AF.Silu, [], [scc.b])
    m_persist = SB.mark()

    def adaln(l, want_ctx_gt):
        lay = {}
        modT = SB.alloc([128, 48, 2], F32)
        vecs = SB.alloc([128, 28], F32)
        LD(vecs[:], vecs_in[l], vecs)
        convw = SB.alloc([128, 16, 3], F32)
        LD(convw[:], convw_in[l], convw)
        s1 = SB.alloc([128, 8, 2], F32)
        s2 = SB.alloc([128, 8, 2], F32)
        gtb = {}
        for nm in (("gt1x", "gt2x", "gt1c", "gt2c") if want_ctx_gt else ("gt1x", "gt2x")):
            gtb[nm] = SB.alloc([128, D], F32)
        m0 = SB.mark()
        brow = SB.alloc([1, 6 * D], F32)
        LD(brow[:], b_ada[l], brow)
        rep = SB.alloc([128, 2, 8, 128], F32)
        for v in range(2):
            for k in range(8):
                TS("dve", rep[:, v, k, :], ones, scc[:, k, v:v + 1], None, ALU.mult, None, [cm.b, scc.b], [rep.b])
        wblk = [SB.alloc([128, 8, 512], F32) for _ in range(2)]
        for j in range(12):
            wb = wblk[j % 2]
            LD(wb[:], w_ada[l, :, j * 512:(j + 1) * 512].rearrange("(k p) n -> p k n", p=128), wb)
            bank = bankrot.next()
            for c in range(4):
                for k in range(8):
                    MM(bank, bank[:, c * 2:c * 2 + 2], wb[:, k, c * 128:(c + 1) * 128], scc[:, k, :], k == 0, False,
                       [wb.b, scc.b])
                MM(bank, bank[:, c * 2:c * 2 + 2], brow[0:1, j * 512 + c * 128:j * 512 + (c + 1) * 128], ones[0:1, 0:2],
                   False, True, [brow.b, cm.b])
            CP("dve", modT[:, j * 4:(j + 1) * 4, :], bank[:, 0:8].rearrange("p (c v) -> p c v", v=2), [], [bank.b, modT.b])
            which = {4: ("gt1", 0), 5: ("gt1", 1), 10: ("gt2", 0), 11: ("gt2", 1)}.get(j)
            if which is not None:
                for v, sfx in ((0, "x"), (1, "c")):
                    nm = which[0] + sfx
                    if nm not in gtb:
                        continue
                    bank = bankrot.next()
                    for k in range(8):
                        MM(bank, bank[:, :], rep[:, v, k, :], wb[:, k, :], k == 0, False, [rep.b, wb.b])
                    MM(bank, bank[:, :], ones[0:1, :], brow[0:1, j * 512:(j + 1) * 512], False, True, [cm.b, brow.b])
                    CP("act", gtb[nm][:, which[1] * 512:(which[1] + 1) * 512], bank[:, :], [], [bank.b, gtb[nm].b])
        for v in range(2):
            TS("dve", s1[:, :, v], modT[:, 8:16, v], 1.0, None, ALU.add, None, [modT.b], [s1.b])
            TT_("dve", s1[:, :, v], s1[:, :, v], vecs[:, 0:8], ALU.mult, [s1.b, vecs.b], [s1.b])
            TS("dve", s2[:, :, v], modT[:, 32:40, v], 1.0, None, ALU.add, None, [modT.b], [s2.b])
            TT_("dve", s2[:, :, v], s2[:, :, v], vecs[:, 16:24], ALU.mult, [s2.b, vecs.b], [s2.b])
        P.barrier()
        SB.release(m0)
        lay.update(modT=modT, vecs=vecs, convw=convw, s1=s1, s2=s2, gtb=gtb)
        return lay

    def norm_transpose(src_tile, reads_src, sc_ap, bi_ap, out_fn, tmp):
        ss, xn = tmp
        ACT(xn[:], src_tile, AF.Square, reads_src, [xn.b, ss.b], accum_out=ss[:, 0:1])
        ACT(ss[:, 1:2], ss[:, 0:1], AF.Sqrt, [epsc.b], [ss.b], scale=1.0 / D, bias=epsc[:, 0:1])
        P.op("dve", lambda e: e.reciprocal(out=ss[:, 1:2], in_=ss[:, 1:2]), [], [ss.b])
        TS("dve", xn[:], src_tile, ss[:, 1:2], None, ALU.mult, None, list(reads_src) + [ss.b], [xn.b])
        for hf in range(debug.get("nhf", 2)):
            bank = bankrot.next()
            for i in range(4):
                k = hf * 4 + i
                TR(bank, bank[:, i * 128:(i + 1) * 128], xn[:, k * 128:(k + 1) * 128], ident, [xn.b, cm.b])
            for i in range(4):
                k = hf * 4 + i
                o_ap, o_bufs = out_fn(k)
                sc, scb = sc_ap(k)
                bi, bib = bi_ap(k)
                if i % 2 == 0:
                    ACT(o_ap, bank[:, i * 128:(i + 1) * 128], AF.Identity, [scb, bib], [bank.b] + list(o_bufs), scale=sc, bias=bi)
                else:
                    TS("dve", o_ap, bank[:, i * 128:(i + 1) * 128], sc, bi, ALU.mult, ALU.add, [scb, bib], [bank.b] + list(o_bufs))

    def phase_proj(l, lay, seq, h_src):
        NT = seq["NT"]
        TT = NT // 128
        var = seq["var"]
        scr = seq["scr"]
        TK = min(512, NT)
        nTK = NT // TK
        mA = SB.mark()
        uT = SB.alloc([128, 8, NT], BF16)
        gsb = seq["gsb"]
        m1 = SB.mark()
        xts = Rot([SB.alloc([128, D], F32) for _ in range(3)])
        tmps = Rot([(SB.alloc([128, 2], F32), SB.alloc([128, D], F32)) for _ in range(2)])
        for t in range(TT if NT == CTX else debug.get("a1_tiles", TT)):
            xt = xts.next()
            LD(xt[:], h_src[t * 128:(t + 1) * 128, :], xt, reads=[seq["hbuf"]])
            norm_transpose(xt[:], [xt.b], lambda k: (lay["s1"][:, k, var:var + 1], lay["s1"].b),
                           lambda k: (lay["modT"][:, k, var:var + 1], lay["modT"].b),
                           lambda k, t=t: (uT[:, k, t * 128:(t + 1) * 128], [uT.b]), tmps.next())
        P.barrier()
        SB.release(m1)
        if debug.get("sub") == "A1" and NT == SEQ:
            return
        wrot = Rot([SB.alloc([128, 8, 512], BF16) for _ in range(2)])

        def load_w(col0, ncols=512):
            wb = wrot.next()
            LD(wb[:, :, 0:ncols], w_in[l, :, col0:col0 + ncols].rearrange("(k p) n -> p k n", p=128), wb, q="pool")
            return wb

        def fm_chunk(wb, c, evac):
            for tk in range(nTK):
                bank = bankrot.next()
                for k in range(8):
                    MM(bank, bank[:, 0:TK], wb[:, k, c * 128:(c + 1) * 128], uT[:, k, tk * TK:(tk + 1) * TK], k == 0, k == 7,
                       [wb.b, uT.b])
                evac(bank, tk)

        m2 = SB.mark()
        zcs = Rot([SB.alloc([128, NT + 2], F32) for _ in range(2)])
        for z in zcs.items:
            MS("dve", z[:], 0.0, [z.b])
        caccs = Rot([SB.alloc([128, NT], F32) for _ in range(2)])
        qkbfs = Rot([SB.alloc([128, NT], BF16) for _ in range(1)])
        ktsts = Rot([SB.alloc([128, 4, 128], BF16) for _ in range(4)])
        convw = lay["convw"]
        def post_chunk(cg, zc):
            if True:
                cacc = caccs.next()
                TS("dve", cacc[:], zc[:, 1:NT + 1], convw[:, cg, 1:2], None, ALU.mult, None, [zc.b, convw.b], [cacc.b])
                STT("dve", cacc[:], zc[:, 0:NT], convw[:, cg, 0:1], cacc[:], ALU.mult, ALU.add, [zc.b, convw.b], [cacc.b])
                STT("dve", cacc[:], zc[:, 2:NT + 2], convw[:, cg, 2:3], cacc[:], ALU.mult, ALU.add, [zc.b, convw.b], [cacc.b])
                qb = qkbfs.next()
                if cg < 8:
                    ACT(qb[:], cacc[:], AF.Silu, [cacc.b], [qb.b])
                    ST(scr["qT"][cg * 128:(cg + 1) * 128, :], qb[:], qb, B("qT", var))
                else:
                    kc = cg - 8
                    ACT(cacc[:], cacc[:], AF.Silu, [], [cacc.b])
                    CP("dve", qb[:], cacc[:], [cacc.b], [qb.b])
                    ST(scr["kT"][kc * 128:(kc + 1) * 128, :], qb[:], qb, B("kT", var))
                    for t0 in range(0, TT, 4):
                        nt = min(4, TT - t0)
                        bank = bankrot.next()
                        for i in range(nt):
                            TR(bank, bank[:, i * 128:(i + 1) * 128], cacc[:, (t0 + i) * 128:(t0 + i + 1) * 128], ident,
                               [cacc.b, cm.b])
                        kst = ktsts.next()
                        CP("act", kst[:, 0:nt, :], bank[:, 0:nt * 128].rearrange("p (t d) -> p t d", d=128), [],
                           [bank.b, kst.b])
                        ST(scr["ktm"][t0 * 128:(t0 + nt) * 128, kc * 128:(kc + 1) * 128].rearrange("(t p) d -> p t d", p=128),
                           kst[:, 0:nt, :], kst, B("ktm", var))

        prev = None
        for blk in range(4):
            wb = load_w(blk * 512)
            for c in range(4):
                cg = blk * 4 + c
                zc = zcs.next()
                fm_chunk(wb, c, lambda bank, tk, zc=zc: CP("act", zc[:, 1 + tk * TK:1 + (tk + 1) * TK], bank[:, 0:TK], [],
                                                          [bank.b, zc.b]))
                if prev is not None:
                    post_chunk(*prev)
                prev = (cg, zc)
        post_chunk(*prev)
        P.barrier()
        SB.release(m2)
        if debug.get("sub") == "A2a" and NT == SEQ:
            return
        m3 = SB.mark()
        wif = SB.alloc([128, 8, 16], BF16)
        LD(wif[:], w_if[l].rearrange("(k p) n -> p k n", p=128), wif, q="pool")
        bif = SB.alloc([1, 16], F32)
        LD(bif[:], b_if[l], bif)
        vsts = Rot([SB.alloc([128, 512], BF16) for _ in range(3)])
        for blk in range(2):
            wb = load_w(V_OFF + blk * 512)
            for t in range(TT):
                bank = bankrot.next()
                for k in range(8):
                    MM(bank, bank[:, :], uT[:, k, t * 128:(t + 1) * 128], wb[:, k, :], k == 0, k == 7, [uT.b, wb.b])
                vst = vsts.next()
                CP("act" if t % 2 == 0 else "dve", vst[:], bank[:, :], [], [bank.b, vst.b])
                ST(scr["vtm"][t * 128:(t + 1) * 128, blk * 512:(blk + 1) * 512], vst[:], vst, B("vtm", var))
        for t0 in range(0, TT, 16):
            nt = min(16, TT - t0)
            bank = bankrot.next()
            for i in range(nt):
                t = t0 + i
                for k in range(8):
                    MM(bank, bank[:, i * 16:(i + 1) * 16], uT[:, k, t * 128:(t + 1) * 128], wif[:, k, :], k == 0, False,
                       [uT.b, wif.b])
                MM(bank, bank[:, i * 16:(i + 1) * 16], ones[0:1, :], bif[0:1, :], False, True, [cm.b, bif.b])
            CP("dve", gsb[:, t0:t0 + nt, :], bank[:, 0:nt * 16].rearrange("p (t g) -> p t g", g=16), [], [bank.b, gsb.b])
        sgs = Rot([SB.alloc([128, TK], BF16) for _ in range(3)])

        def sig_block(col0, dst, dname):
            for blk in range(2):
                wb = load_w(col0 + blk * 512)
                for c in range(4):
                    cg = blk * 4 + c

                    def ev(bank, tk, cg=cg):
                        sg = sgs.next()
                        ACT(sg[:], bank[:, 0:TK], AF.Sigmoid, [], [bank.b, sg.b])
                        ST(dst[cg * 128:(cg + 1) * 128, tk * TK:(tk + 1) * TK], sg[:], sg, B(dname, var))
                    fm_chunk(wb, c, ev)

        if seq["with_out"]:
            sig_block(O_OFF, scr["oT"], "oT")
            sig_block(GA_OFF, scr["gaT"], "gaT")
            sig_block(GB_OFF, scr["gbT"], "gbT")
        P.barrier()
        SB.release(m3)
        if debug.get("sub") == "A2b" and NT == SEQ:
            return
        if seq["with_out"]:
            m4 = SB.mark()
            wpl = SB.alloc([128, 4, 128], BF16)
            LD(wpl[:], w_pool[l].rearrange("g c e -> c g e"), wpl, q="pool")
            wb = load_w(POOL_OFF)
            pz = SB.alloc([128, NT], F32)
            a1 = SB.alloc([128, NT], F32)
            a2 = SB.alloc([128, NT], F32)
            ivc = SB.alloc([128, NT], F32)
            pmb = SB.alloc([128, NT], BF16)
            sgs = Rot([SB.alloc([128, TK], BF16) for _ in range(3)])
            invc_src = invc_ctx if NT == CTX else invc_lat
            for g in range(4):
                w = POOL_W[g]
                LD(ivc[:], invc_src[g], ivc)
                fm_chunk(wb, g, lambda bank, tk: CP("act", pz[:, tk * TK:(tk + 1) * TK], bank[:, 0:TK], [], [bank.b, pz.b]))
                if NT == CTX:
                    CP("dve", a2[:], pz[:], [pz.b], [a2.b])
                    for o in range(-(w // 2), w // 2):
                        if o == 0:
                            continue
                        lo, hi = max(0, -o), min(NT, NT - o)
                        TT_("dve", a2[:, lo:hi], a2[:, lo:hi], pz[:, lo + o:hi + o], ALU.add, [pz.b], [a2.b])
                else:
                    G = 64
                    pz3 = pz[:].rearrange("p (r x) -> p r x", x=G)
                    a13 = a1[:].rearrange("p (r x) -> p r x", x=G)
                    CP("dve", a1[:], pz[:], [pz.b], [a1.b])
                    for o in range(-(w // 2), w // 2):
                        if o == 0:
                            continue
                        lo, hi = max(0, -o), min(G, G - o)
                        TT_("dve", a13[:, :, lo:hi], a13[:, :, lo:hi], pz3[:, :, lo + o:hi + o], ALU.add, [pz.b], [a1.b])
                    CP("dve", a2[:], a1[:], [a1.b], [a2.b])
                    for o in range(-(w // 2), w // 2):
                        if o == 0:
                            continue
                        lo, hi = max(0, -o), min(G, G - o)
                        TT_("dve", a2[:, lo * G:hi * G], a2[:, lo * G:hi * G], a1[:, (lo + o) * G:(hi + o) * G], ALU.add,
                            [a1.b], [a2.b])
                TT_("dve", a2[:], a2[:], ivc[:], ALU.mult, [ivc.b], [a2.b])
                TT_("dve", pmb[:], a2[:], pz[:], ALU.subtract, [a2.b, pz.b], [pmb.b])
                for tk in range(nTK):
                    bank = bankrot.next()
                    MM(bank, bank[:, 0:TK], wpl[:, g, :], pmb[:, tk * TK:(tk + 1) * TK], True, True, [wpl.b, pmb.b])
                    sg = sgs.next()
                    ACT(sg[:], bank[:, 0:TK], AF.Identity, [lay["vecs"].b], [bank.b, sg.b], scale=lay["vecs"][:, 24 + g:25 + g])
                    ST(scr["pmwT"][g * 128:(g + 1) * 128, tk * TK:(tk + 1) * TK], sg[:], sg, B("pmwT", var))
            P.barrier()
            SB.release(m4)
        P.barrier()
        SB.release(mA)

    def gate_prep(seq):
        TT = seq["NT"] // 128
        gsb = seq["gsb"]
        gd = seq["gd"]
        lf, bc, a_s, wk, ebl = gd["lf"], gd["bc"], gd["a_s"], gd["wk"], gd["ebl"]
        gf = gsb[:, :, 8:16]
        ACT(a_s[:], gf, AF.Abs, [gsb.b], [a_s.b])
        ACT(a_s[:], a_s[:], AF.Exp, [], [a_s.b], scale=-1.0)
        ACT(a_s[:], a_s[:], AF.Ln, [epsc.b], [a_s.b], bias=epsc[:, 1:2])
        TS("dve", lf[:], gf, 0.0, None, ALU.min, None, [gsb.b], [lf.b])
        TT_("dve", lf[:], lf[:], a_s[:], ALU.subtract, [a_s.b], [lf.b])
        for d in range(2):
            bank = bankrot.next()
            for t in range(TT):
                MM(bank, bank[:, t * 4:(t + 1) * 4], tri[d], lf[:, t, d * 4:(d + 1) * 4], True, True, [cm.b, lf.b])
            CP("dve", bc[:, :, d * 4:(d + 1) * 4], bank[:, 0:TT * 4].rearrange("p (t h) -> p t h", h=4), [], [bank.b, bc.b])
        bank = bankrot.next()
        MM(bank, bank[:, 0:TT * 8], ones, lf[:].rearrange("p t g -> p (t g)"), True, True, [cm.b, lf.b])
        CP("dve", ebl[:].rearrange("p t g -> p (t g)"), bank[:, 0:TT * 8], [], [bank.b, ebl.b])
        TT_("dve", a_s[:], gsb[:, :, 0:8], bc[:], ALU.subtract, [gsb.b, bc.b], [a_s.b])
        TT_("dve", wk[:], a_s[:], ebl[:], ALU.add, [a_s.b, ebl.b], [wk.b])
        ACT(wk[:], wk[:], AF.Exp, [epsc.b], [wk.b], bias=epsc[:, 2:3])
        ACT(ebl[:], ebl[:], AF.Exp, [wk.b], [ebl.b])

    def scan_chunk(seq, t, d, data, st, work, with_out, htile):
        gsb, gd = seq["gsb"], seq["gd"]
        lf, bc, wk, ebl = gd["lf"], gd["bc"], gd["wk"], gd["ebl"]
        qTt, kTt, ktmt, vt = data
        Cf, Cb = st
        H = range(NH)
        W = [dict() for _ in H]
        for h in H:
            dh = d * 4 + h
            w = W[h]
            w["dh"] = dh
            w["vw"] = work["vw"].next()
            ACT(w["vw"][:], vt[:, h, :], AF.Identity, [vt.b, wk.b], [w["vw"].b], scale=wk[:, t, dh:dh + 1])
            if with_out:
                for nm in ("lfrep", "arg", "dec", "ebr", "sdT", "qs", "den"):
                    w[nm] = work[nm].next()
                ACT(w["lfrep"][:], ones, AF.Identity, [cm.b, lf.b], [w["lfrep"].b], scale=lf[:, t, dh:dh + 1])
        for h in H:
            W[h]["bx"] = bankrot.next()
        for h in H:
            W[h]["bw"] = bankrot.next()
        for h in H:
            w = W[h]
            dh = w["dh"]
            bx = w["bx"]
            if with_out:
                MM(bx, bx[:, 0:128], w["lfrep"][:], tri[d], True, True, [w["lfrep"].b, cm.b])
                for dc in range(2):
                    MM(bx, bx[:, 128:256], kTt[:, h * 2 + dc, :], qTt[:, h * 2 + dc, :], dc == 0, dc == 1, [kTt.b, qTt.b])
            bw = w["bw"]
            vw = w["vw"]
            for dc in range(2):
                MM(bw, bw[:, dc * 256:(dc + 1) * 256], ktmt[:, h * 256 + dc * 128:h * 256 + (dc + 1) * 128], vw[:, 0:256], True, True,
                   [ktmt.b, vw.b])
            for dc in range(2):
                MM(bx, bx[:, 256 + dc:257 + dc], ktmt[:, h * 256 + dc * 128:h * 256 + (dc + 1) * 128], vw[:, 256:257], True, True,
                   [ktmt.b, vw.b])
        def n_update(h):
            w = W[h]
            dh, bx = w["dh"], w["bx"]
            STT("dve", Cf[:, dh, :, 256:257], Cf[:, dh, :, 256:257], ebl[:, t, dh:dh + 1],
                bx[:, 256:258].rearrange("p (c e) -> p c e", c=2), ALU.mult, ALU.add, [ebl.b], [bx.b, B("Cf", dh)])

        if with_out:
            for h in H:
                w = W[h]
                dh, bx = w["dh"], w["bx"]
                STT("dve", w["arg"][:], bx[:, 0:128], bc[:, t, dh:dh + 1], negm[d], ALU.subtract, ALU.add, [bc.b, cm.b],
                    [bx.b, w["arg"].b])
                ACT(w["ebr"][:], bx[:, 0:128], AF.Exp, [], [bx.b, w["ebr"].b])
            for h in H:
                w = W[h]
                ACT(w["dec"][:], w["arg"][:], AF.Exp, [w["arg"].b, gsb.b], [w["dec"].b], bias=gsb[:, t, w["dh"]:w["dh"] + 1])
            for h in H:
                w = W[h]
                bx = w["bx"]
                STT("dve", w["sdT"][:], bx[:, 128:256], 1.0 / 16.0, w["dec"][:], ALU.mult, ALU.mult, [w["dec"].b], [bx.b, w["sdT"].b])
                n_update(h)
                for dc in range(2):
                    TT_("dve", w["qs"][:, dc, :], qTt[:, h * 2 + dc, :], w["ebr"][:], ALU.mult, [qTt.b, w["ebr"].b], [w["qs"].b])
        for h in H:
            w = W[h]
            dh, bw = w["dh"], w["bw"]
            STT("dve", Cf[:, dh, :, 0:256], Cf[:, dh, :, 0:256], ebl[:, t, dh:dh + 1], bw[:, :].rearrange("p (c e) -> p c e", c=2),
                ALU.mult, ALU.add, [ebl.b], [bw.b, B("Cf", dh)])
        if not with_out:
            for h in H:
                n_update(h)
        if with_out:
            for h in H:
                w = W[h]
                dh = w["dh"]
                bz = w["bz"] = bankrot.next()
                for dc in range(2):
                    MM(bz, bz[:, 0:257], w["qs"][:, dc, :], Cb[:, dh, dc, :], dc == 0, False, [w["qs"].b, B("Cb", dh)])
                MM(bz, bz[:, 0:257], w["sdT"][:], vt[:, h, :], False, True, [w["sdT"].b, vt.b])
        if with_out:
            for h in H:
                w = W[h]
                ACT(w["den"][:, 0:1], w["bz"][:, 256:257], AF.Abs, [], [w["bz"].b, w["den"].b])
        for h in H:
            dh = W[h]["dh"]
            CP("act", Cb[:, dh, :, :], Cf[:, dh, :, :], [B("Cf", dh)], [B("Cb", dh)])
        if with_out:
            for h in H:
                den = W[h]["den"]
                TS("dve", den[:, 0:1], den[:, 0:1], 1.0, None, ALU.max, None, [], [den.b])
            for h in H:
                den = W[h]["den"]
                P.op("dve", lambda e, den=den: e.reciprocal(out=den[:, 0:1], in_=den[:, 0:1]), [], [den.b])
            for h in H:
                w = W[h]
                ACT(htile[:, h * 256:(h + 1) * 256], w["bz"][:, 0:256], AF.Identity, [w["den"].b], [w["bz"].b, htile.b],
                    scale=w["den"][:, 0:1])

    def phase_scan(l, lay, seq, h_src, last_layer):
        NT = seq["NT"]
        TT = NT // 128
        var = seq["var"]
        scr = seq["scr"]
        with_out = seq["with_out"]
        Cf, Cb = seq["Cf"], seq["Cb"]
        gate_prep(seq)
        mB = SB.mark()
        work = {
            "lfrep": Rot([SB.alloc([128, 128], F32) for _ in range(4)]),
            "arg": Rot([SB.alloc([128, 128], F32) for _ in range(4)]),
            "dec": Rot([SB.alloc([128, 128], F32) for _ in range(4)]),
            "ebr": Rot([SB.alloc([128, 128], BF16) for _ in range(4)]),
            "sdT": Rot([SB.alloc([128, 128], BF16) for _ in range(4)]),
            "qs": Rot([SB.alloc([128, 2, 128], BF16) for _ in range(4)]),
            "den": Rot([SB.alloc([128, 2], F32) for _ in range(4)]),
            "vw": Rot([SB.alloc([128, 257], BF16) for _ in range(4)]),
        }
        dq = Rot([SB.alloc([128, 8, 128], BF16) for _ in range(2)])
        dk = Rot([SB.alloc([128, 8, 128], BF16) for _ in range(2)])
        dkt = Rot([SB.alloc([128, D], BF16) for _ in range(2)])
        dv = Rot([SB.alloc([128, 4, 257], BF16) for _ in range(2)])
        for v_ in dv.items:
            MS("dve", v_[:], 1.0, [v_.b])

        def load_chunk(t):
            qTt, kTt, ktmt, vt = dq.next(), dk.next(), dkt.next(), dv.next()
            if with_out:
                LD(qTt[:], scr["qT"][:, t * 128:(t + 1) * 128].rearrange("(c p) t -> p c t", p=128), qTt, reads=[B("qT", var)])
                LD(kTt[:], scr["kT"][:, t * 128:(t + 1) * 128].rearrange("(c p) t -> p c t", p=128), kTt, reads=[B("kT", var)])
            LD(ktmt[:], scr["ktm"][t * 128:(t + 1) * 128, :], ktmt, reads=[B("ktm", var)])
            LD(vt[:, :, 0:256], scr["vtm"][t * 128:(t + 1) * 128, :].rearrange("p (h e) -> p h e", e=256), vt,
               reads=[B("vtm", var)])
            return (qTt, kTt, ktmt, vt)

        hfs = Rot([SB.alloc([128, D], F32) for _ in range(2)])
        nxt = load_chunk(0)
        for t in range(TT):
            data = nxt
            if t + 1 < TT:
                nxt = load_chunk(t + 1)
            hft = hfs.next()
            scan_chunk(seq, t, 0, data, (Cf, Cb), work, with_out, hft)
            if with_out:
                ST(scr["hf"][t * 128:(t + 1) * 128, :], hft[:], hft, B("hf", var))
        if not with_out:
            nxt = load_chunk(TT - 1)
            for t in range(TT - 1, -1, -1):
                data = nxt
                if t > 0:
                    nxt = load_chunk(t - 1)
                scan_chunk(seq, t, 1, data, (Cf, Cb), work, False, None)
            P.barrier()
            SB.release(mB)
            return
        wpa = SB.alloc([128, 8, D], BF16)
        LD(wpa[:], w_pa[l].rearrange("(k p) n -> p k n", p=128), wpa, q="pool")
        wpb = SB.alloc([128, 4, D], BF16)
        LD(wpb[:], w_pb[l].rearrange("(k p) n -> p k n", p=128), wpb, q="pool")
        wo = SB.alloc([128, 8, D], BF16)
        LD(wo[:], w_out[l].rearrange("(k p) n -> p k n", p=128), wo, q="pool")
        GT = min(4, TT)
        GN = GT * 128
        hbs = Rot([SB.alloc([128, D], F32) for _ in range(1)])
        stats = SB.alloc([128, 4, 6], F32)
        mv = SB.alloc([128, 4, 2], F32)
        hmT = SB.alloc([128, 8, GN], BF16)
        oTt = SB.alloc([128, 8, GN], BF16)
        gaTt = SB.alloc([128, 8, GN], BF16)
        gbTt = SB.alloc([128, 8, GN], BF16)
        pmTt = SB.alloc([128, 4, GN], BF16)
        mT = SB.alloc([128, 8, GN], BF16)
        t1s = Rot([SB.alloc([128, GN], F32) for _ in range(2)])
        t2s = Rot([SB.alloc([128, GN], F32) for _ in range(1)])
        hos = Rot([SB.alloc([128, D], F32) for _ in range(1)])
        hns = Rot([SB.alloc([128, D], F32) for _ in range(2)])
        tmps = Rot([(SB.alloc([128, 2], F32), SB.alloc([128, D], F32)) for _ in range(2)])
        u2s = Rot([SB.alloc([128, 8, 128], BF16) for _ in range(2)])
        u2f = SB.alloc([128, 8, 128], F32) if last_layer else None
        if last_layer:
            wr = SB.alloc([128, 8, NE], F32)
            LD(wr[:], w_router[0].rearrange("(k p) n -> p k n", p=128), wr)
            mx = SB.alloc([128, 8], F32)
            lg = SB.alloc([128, 8], F32)
            msk = SB.alloc([128, 8], F32)
            sm = SB.alloc([128, 4], F32)
        gt1 = lay["gtb"]["gt1" + ("x" if var == 0 else "c")]
        vecs = lay["vecs"]
        def load_chunk_b(t):
            dat = load_chunk(t)
            hf_ = hfs.next()
            LD(hf_[:], scr["hf"][t * 128:(t + 1) * 128, :], hf_, reads=[B("hf", var)])
            return dat, hf_

        def tail_tile(g, ti):
            t = g * GT + ti
            ho, hn = hos.next(), hns.next()
            LD(ho[:], h_src[t * 128:(t + 1) * 128, :], ho, reads=[seq["hbuf"]])
            for hf in range(2):
                bank = bankrot.next()
                for k in range(8):
                    MM(bank, bank[:, :], mT[:, k, ti * 128:(ti + 1) * 128], wo[:, k, hf * 512:(hf + 1) * 512], k == 0, k == 7,
                       [mT.b, wo.b])
                TT_("dve", hn[:, hf * 512:(hf + 1) * 512], bank[:, :], gt1[:, hf * 512:(hf + 1) * 512], ALU.mult, [gt1.b],
                    [bank.b, hn.b])
            TT_("dve", hn[:], hn[:], ho[:], ALU.add, [ho.b], [hn.b])
            ST(scr["h1"][t * 128:(t + 1) * 128, :], hn[:], hn, B("h1", var), q="pool")
            u2 = u2s.next()
            if last_layer:
                norm_transpose(hn[:], [hn.b], lambda k: (lay["s2"][:, k, var:var + 1], lay["s2"].b),
                               lambda k: (lay["modT"][:, 24 + k, var:var + 1], lay["modT"].b),
                               lambda k: (u2f[:, k, :], [u2f.b]), tmps.next())
                CP("act", u2[:], u2f[:], [u2f.b], [u2.b])
                bank = bankrot.next()
                for k in range(8):
                    MM(bank, bank[:, 0:NE], u2f[:, k, :], wr[:, k, :], k == 0, k == 7, [u2f.b, wr.b])
                CP("dve", lg[:], bank[:, 0:NE], [], [bank.b, lg.b])
                P.op("dve", lambda e: e.max(out=mx[:], in_=lg[:]), [lg.b], [mx.b])
                TS("dve", msk[:], lg[:], mx[:, 1:2], None, ALU.is_ge, None, [lg.b, mx.b], [msk.b])
                TS("dve", sm[:, 0:1], mx[:, 0:1], -1.0, None, ALU.mult, None, [mx.b], [sm.b])
                ACT(lg[:], lg[:], AF.Exp, [sm.b], [lg.b], bias=sm[:, 0:1])
                ACT(sm[:, 1:2], mx[:, 1:2], AF.Exp, [sm.b, mx.b], [sm.b], bias=sm[:, 0:1])
                TS("dve", sm[:, 1:2], sm[:, 1:2], 1.0, None, ALU.add, None, [], [sm.b])
                P.op("dve", lambda e: e.reciprocal(out=sm[:, 1:2], in_=sm[:, 1:2]), [], [sm.b])
                TT_("dve", lg[:], lg[:], msk[:], ALU.mult, [msk.b], [lg.b])
                TS("dve", seq["moeg"][:, t, :], lg[:], sm[:, 1:2], None, ALU.mult, None, [lg.b, sm.b], [seq["moeg"].b])
            else:
                norm_transpose(hn[:], [hn.b], lambda k: (lay["s2"][:, k, var:var + 1], lay["s2"].b),
                               lambda k: (lay["modT"][:, 24 + k, var:var + 1], lay["modT"].b),
                               lambda k: (u2[:, k, :], [u2.b]), tmps.next())
            ST(scr["u2T"][:, t * 128:(t + 1) * 128].rearrange("(c p) t -> p c t", p=128), u2[:], u2, B("u2T", var), q="pool")

        nxt = load_chunk_b(TT - 1)
        pending = []
        for g in range(TT // GT - 1, -1, -1):
            T0 = g * GN
            LD(oTt[:], scr["oT"][:, T0:T0 + GN].rearrange("(c p) t -> p c t", p=128), oTt, q="pool", reads=[B("oT", var)])
            LD(gaTt[:], scr["gaT"][:, T0:T0 + GN].rearrange("(c p) t -> p c t", p=128), gaTt, q="pool", reads=[B("gaT", var)])
            LD(gbTt[:], scr["gbT"][:, T0:T0 + GN].rearrange("(c p) t -> p c t", p=128), gbTt, q="pool", reads=[B("gbT", var)])
            LD(pmTt[:], scr["pmwT"][:, T0:T0 + GN].rearrange("(c p) t -> p c t", p=128), pmTt, q="pool", reads=[B("pmwT", var)])
            for ti in range(GT - 1, -1, -1):
                t = g * GT + ti
                data, hft = nxt
                if t > 0:
                    nxt = load_chunk_b(t - 1)
                hbt = hbs.next()
                scan_chunk(seq, t, 1, data, (Cf, Cb), work, True, hbt)
                TT_("dve", hbt[:], hbt[:], hft[:], ALU.add, [hft.b], [hbt.b])
                for h in range(NH):
                    P.op("dve", lambda e, h=h, hbt=hbt: e.bn_stats(out=stats[:, h, :], in_=hbt[:, h * 256:(h + 1) * 256]),
                         [hbt.b], [stats.b])
                    P.op("dve", lambda e, h=h: e.bn_aggr(out=mv[:, h, :], in_=stats[:, h, :]), [stats.b], [mv.b])
                ACT(mv[:, :, 1], mv[:, :, 1], AF.Sqrt, [epsc.b], [mv.b], bias=epsc[:, 0:1])
                P.op("dve", lambda e: e.reciprocal(out=mv[:, :, 1], in_=mv[:, :, 1]), [], [mv.b])
                for h in range(NH):
                    TS("dve", hbt[:, h * 256:(h + 1) * 256], hbt[:, h * 256:(h + 1) * 256], mv[:, h, 0:1], mv[:, h, 1:2],
                       ALU.subtract, ALU.mult, [mv.b], [hbt.b])
                for hf in range(2):
                    bank = bankrot.next()
                    for i in range(4):
                        k = hf * 4 + i
                        TR(bank, bank[:, i * 128:(i + 1) * 128], hbt[:, k * 128:(k + 1) * 128], ident, [hbt.b, cm.b])
                    for i in range(4):
                        k = hf * 4 + i
                        STT("dve", hmT[:, k, ti * 128:(ti + 1) * 128], bank[:, i * 128:(i + 1) * 128], vecs[:, 8 + k:9 + k],
                            oTt[:, k, ti * 128:(ti + 1) * 128], ALU.mult, ALU.mult, [vecs.b, oTt.b], [bank.b, hmT.b])
                if pending:
                    tail_tile(*pending.pop(0))
            while pending:
                tail_tile(*pending.pop(0))
            for f in range(8):
                ba = bankrot.next()
                for k in range(8):
                    MM(ba, ba[:, 0:GN], wpa[:, k, f * 128:(f + 1) * 128], hmT[:, k, :], k == 0, k == 7, [wpa.b, hmT.b])
                bb = bankrot.next()
                for k in range(4):
                    MM(bb, bb[:, 0:GN], wpb[:, k, f * 128:(f + 1) * 128], pmTt[:, k, :], k == 0, k == 3, [wpb.b, pmTt.b])
                t1, t2 = t1s.next(), t2s.next()
                TT_("dve", t1[:], ba[:, 0:GN], gaTt[:, f, :], ALU.mult, [gaTt.b], [ba.b, t1.b])
                TT_("dve", t2[:], bb[:, 0:GN], gbTt[:, f, :], ALU.mult, [gbTt.b], [bb.b, t2.b])
                TT_("dve", mT[:, f, :], t1[:], t2[:], ALU.add, [t1.b, t2.b], [mT.b])
            pending = [(g, ti) for ti in range(GT)]
        while pending:
            tail_tile(*pending.pop(0))
        seq["scan_peak"] = SB.lo
        P.barrier()
        SB.release(mB)

    def phase_ffn(l, lay, seq, experts, nff, h_dst, final):
        NT = seq["NT"]
        var = seq["var"]
        scr = seq["scr"]
        TB = min(1024, NT)
        nTB = NT // TB
        SUBW = min(512, TB)
        nsub = TB // SUBW
        nts = TB // 128
        NJ = nff // 128
        mF = SB.mark()
        u2 = SB.alloc([128, 8, TB], BF16)
        act = SB.alloc([128, NJ, TB], BF16)
        yacc = SB.alloc([128, nts, D], F32)
        wds = Rot([SB.alloc([128, NJ, 512], BF16) for _ in range(2)])
        WB = 128
        wgs = Rot([SB.alloc([128, 8, WB], BF16) for _ in range(2)])
        wus = Rot([SB.alloc([128, 8, WB], BF16) for _ in range(2)])
        sgs = Rot([SB.alloc([128, SUBW], BF16) for _ in range(2)])
        hos = Rot([SB.alloc([128, D], F32) for _ in range(2)])
        gt2 = lay["gtb"]["gt2" + ("x" if var == 0 else "c")]
        if final:
            gfin = SB.alloc([128, D], F32)
            LD(gfin[:], gfin_in, gfin)
            tmpf = Rot([(SB.alloc([128, 2], F32), SB.alloc([128, D], F32)) for _ in range(1)])
        moeg = seq.get("moeg")
        def residual_tile(T0, ts):
            t = T0 // 128 + ts
            ho = hos.next()
            LD(ho[:], scr["h1"][t * 128:(t + 1) * 128, :], ho, reads=[B("h1", var)])
            ybs = [B("yacc", ts, 0), B("yacc", ts, 1)]
            TT_("dve", yacc[:, ts, :], yacc[:, ts, :], gt2[:], ALU.mult, [gt2.b], ybs)
            TT_("dve", ho[:], ho[:], yacc[:, ts, :], ALU.add, ybs, [ho.b])
            if not final:
                ST(h_dst[0][t * 128:(t + 1) * 128, :], ho[:], ho, h_dst[1])
            else:
                ss, xn = tmpf.next()
                ACT(xn[:], ho[:], AF.Square, [ho.b], [xn.b, ss.b], accum_out=ss[:, 0:1])
                ACT(ss[:, 1:2], ss[:, 0:1], AF.Sqrt, [epsc.b], [ss.b], scale=1.0 / D, bias=epsc[:, 0:1])
                P.op("dve", lambda e, ss=ss: e.reciprocal(out=ss[:, 1:2], in_=ss[:, 1:2]), [], [ss.b])
                STT("dve", xn[:], ho[:], ss[:, 1:2], gfin[:], ALU.mult, ALU.mult, [ho.b, ss.b, gfin.b], [xn.b])
                ST(y_out[t * 128:(t + 1) * 128, :], xn[:], xn, B("y"))

        def load_u2(tb_):
            LD(u2[:], scr["u2T"][:, tb_ * TB:(tb_ + 1) * TB].rearrange("(c p) t -> p c t", p=128), u2, reads=[B("u2T", var)])

        load_u2(0)
        pend_res = []
        for tb in range(nTB):
            T0 = tb * TB
            for ei, (wg_ap, wu_ap, wd_ap) in enumerate(experts):
                wd0 = None
                for j0 in range(0, nff, WB):
                    if j0 == 2 * WB:
                        wd0 = wds.next()
                        LD(wd0[:], wd_ap[:, 0:512].rearrange("(j p) n -> p j n", p=128), wd0, q="pool")
                    ncol = min(WB, nff - j0)
                    wg, wu = wgs.next(), wus.next()
                    LD(wg[:, :, 0:ncol], wg_ap[:, j0:j0 + ncol].rearrange("(k p) n -> p k n", p=128), wg, q="pool")
                    LD(wu[:, :, 0:ncol], wu_ap[:, j0:j0 + ncol].rearrange("(k p) n -> p k n", p=128), wu, q="pool")
                    for c in range(ncol // 128):
                        j = j0 // 128 + c
                        for s in range(nsub):
                            bg = bankrot.next()
                            for k in range(8):
                                MM(bg, bg[:, 0:SUBW], wg[:, k, c * 128:(c + 1) * 128], u2[:, k, s * SUBW:(s + 1) * SUBW], k == 0,
                                   k == 7, [wg.b, u2.b])
                            bu = bankrot.next()
                            for k in range(8):
                                MM(bu, bu[:, 0:SUBW], wu[:, k, c * 128:(c + 1) * 128], u2[:, k, s * SUBW:(s + 1) * SUBW], k == 0,
                                   k == 7, [wu.b, u2.b])
                            sg = sgs.next()
                            ACT(sg[:], bg[:, 0:SUBW], AF.Silu, [], [bg.b, sg.b])
                            TT_("dve", act[:, j, s * SUBW:(s + 1) * SUBW], bu[:, 0:SUBW], sg[:], ALU.mult, [sg.b], [bu.b, act.b])
                    if pend_res and (j0 // WB) % 2 == 1:
                        residual_tile(*pend_res.pop(0))
                while pend_res:
                    residual_tile(*pend_res.pop(0))
                if ei == len(experts) - 1 and tb + 1 < nTB:
                    load_u2(tb + 1)
                for hf in range(2):
                    if hf == 0:
                        wd = wd0
                    else:
                        wd = wds.next()
                        LD(wd[:], wd_ap[:, hf * 512:(hf + 1) * 512].rearrange("(j p) n -> p j n", p=128), wd, q="pool")
                    for ts in range(nts):
                        bank = bankrot.next()
                        for j in range(NJ):
                            MM(bank, bank[:, :], act[:, j, ts * 128:(ts + 1) * 128], wd[:, j, :], j == 0, j == NJ - 1, [act.b, wd.b])
                        ya = yacc[:, ts, hf * 512:(hf + 1) * 512]
                        yb = B("yacc", ts, hf)
                        if moeg is None:
                            CP("act", ya, bank[:, :], [], [bank.b, yb])
                        else:
                            tg = (T0 // 128) + ts
                            if ei == 0:
                                ACT(ya, bank[:, :], AF.Identity, [moeg.b], [bank.b, yb], scale=moeg[:, tg, ei:ei + 1])
                            else:
                                STT("dve", ya, bank[:, :], moeg[:, tg, ei:ei + 1], ya, ALU.mult, ALU.add, [moeg.b], [bank.b, yb])
            pend_res = [(T0, ts) for ts in range(nts)]
        while pend_res:
            residual_tile(*pend_res.pop(0))
        P.barrier()
        SB.release(mF)

    stop = debug.get("stop")
    hx_src, hx_buf = x_in, B("h_in_x")
    hc_src, hc_buf = ctx_in, B("h_in_c")
    for l in range(2):
        last = l == 1
        mL = SB.mark()
        lay = adaln(l, want_ctx_gt=not last)
        moeg_t = SB.alloc([128, SEQ // 128, NE], F32) if last else None
        mM = SB.mark()
        Cf = SB.alloc([128, 8, 2, 257], F32)
        Cb = SB.alloc([128, 8, 2, 257], BF16)
        MS("dve", Cf[:], 0.0, [B("Cf", i) for i in range(8)])
        MS("dve", Cb[:], 0.0, [B("Cb", i) for i in range(8)])

        def mkseq(NT, var, scr, with_out, hbuf):
            TT = NT // 128
            s = dict(NT=NT, var=var, scr=scr, with_out=with_out, hbuf=hbuf, Cf=Cf, Cb=Cb)
            s["gsb"] = SB.alloc([128, TT, 16], F32)
            s["gd"] = {n: SB.alloc([128, TT, 8], F32) for n in ("lf", "bc", "a_s", "wk", "ebl")}
            return s

        sc = mkseq(CTX, 1, scr_c, not last, hc_buf)
        sx = mkseq(SEQ, 0, scr_x, True, hx_buf)
        if last:
            sx["moeg"] = moeg_t
        if not debug.get("skip_ctx"):
            phase_proj(l, lay, sc, hc_src)
            phase_scan(l, lay, sc, hc_src, False)
        if stop == ("ctx_scan", l):
            break
        phase_proj(l, lay, sx, hx_src)
        if stop == ("proj", l):
            break
        phase_scan(l, lay, sx, hx_src, last)
        if stop == ("scan", l):
            break
        P.barrier()
        SB.release(mM)
        if not last:
            dense = [(w_ffg[0], w_ffu[0], w_ffd[0])]
            phase_ffn(l, lay, sx, dense, DFF, (scr_x["h2"], B("h2", 0)), False)
            phase_ffn(l, lay, sc, dense, DFF, (scr_c["h2"], B("h2", 1)), False)
            hx_src, hx_buf = scr_x["h2"], B("h2", 0)
            hc_src, hc_buf = scr_c["h2"], B("h2", 1)
        else:
            experts = [(w_eg[0, e], w_eu[0, e], w_ed[0, e]) for e in range(NE)]
            phase_ffn(l, lay, sx, experts, DFE, None, True)
        P.barrier()
        SB.release(mL)
    stats = P.finalize()
    return nc, stats


def _consts():
    i = np.arange(128)
    ident = np.eye(128, dtype=np.float32)
    triF = (i[:, None] <= i[None, :]).astype(np.float32)
    triB = (i[:, None] >= i[None, :]).astype(np.float32)
    negF = np.where(i[:, None] <= i[None, :], 0.0, NEG).astype(np.float32)
    negB = np.where(i[:, None] >= i[None, :], 0.0, NEG).astype(np.float32)
    ones = np.ones((128, 128), np.float32)
    cmat = np.stack([ident, triF, triB, negF, negB, ones]).astype(np.float32)

    def cnt(n, w):
        t = np.arange(n)
        lo = np.clip(t - w // 2, 0, n)
        hi = np.clip(t + w // 2, 0, n)
        return (hi - lo).astype(np.float32)

    lat = []
    cx = []
    for w in POOL_W:
        c1 = cnt(64, w)
        lat.append(np.broadcast_to((1.0 / (c1[:, None] * c1[None, :])).reshape(1, SEQ), (128, SEQ)))
        cx.append(np.broadcast_to((1.0 / cnt(CTX, w)).reshape(1, CTX), (128, CTX)))
    return cmat, np.ascontiguousarray(np.stack(lat), np.float32), np.ascontiguousarray(np.stack(cx), np.float32)


def _pk(v, k):
    return np.ascontiguousarray(np.asarray(v, np.float32).reshape(k, 128).T)


def prep_inputs(inputs):
    f = lambda a: np.ascontiguousarray(np.asarray(a, dtype=np.float32))
    x, c, ctx, c_ctx = f(inputs["x"]), f(inputs["c"]), f(inputs["ctx"]), f(inputs["c_ctx"])
    w_in = f(inputs["w_in"])
    cmat, invc_lat, invc_ctx = _consts()
    perm = [d * 8 + g * 4 + h for g in range(2) for d in range(2) for h in range(4)]
    w_if = np.ascontiguousarray(w_in[:, :, IF_OFF:IF_OFF + 16][:, :, perm])
    b_if = np.ascontiguousarray(f(inputs["b_if"]).transpose(0, 2, 1, 3).reshape(2, 1, 16))
    vecs = np.stack([np.concatenate([_pk(inputs["g_mix"][l], 8), _pk(inputs["head_gain"][l], 8), _pk(inputs["g_ffn"][l], 8),
                                     _pk(inputs["pool_scale"][l], 4)], axis=1) for l in range(2)])
    convw = np.stack([np.ascontiguousarray(f(inputs["conv_qk"])[l].reshape(3, 16, 128).transpose(2, 1, 0)) for l in range(2)])
    shared = {
        "w_ada": f(inputs["w_ada"]), "b_ada": f(inputs["b_ada"]).reshape(2, 1, 6 * D),
        "vecs": np.ascontiguousarray(vecs, np.float32), "convw": np.ascontiguousarray(convw, np.float32),
        "w_in": w_in, "w_if": w_if, "b_if": b_if,
        "w_pool": f(inputs["w_pool"]), "w_pa": f(inputs["w_pa"]), "w_pb": f(inputs["w_pb"]), "w_out": f(inputs["w_out"]),
        "w_ff_gate": f(inputs["w_ff_gate"]), "w_ff_up": f(inputs["w_ff_up"]), "w_ff_down": f(inputs["w_ff_down"]),
        "w_router": f(inputs["w_router"]), "w_exp_gate": f(inputs["w_exp_gate"]), "w_exp_up": f(inputs["w_exp_up"]),
        "w_exp_down": f(inputs["w_exp_down"]),
        "g_final_b": np.ascontiguousarray(np.broadcast_to(f(inputs["g_final"])[None, :], (128, D))),
        "cmat": cmat, "invc_lat": invc_lat, "invc_ctx": invc_ctx,
    }
    in_maps = []
    for b in range(8):
        m = dict(shared)
        m["x"] = x[b]
        m["ctx"] = ctx[b]
        m["cc"] = np.ascontiguousarray(np.stack([_pk(c[b], 8), _pk(c_ctx, 8)], axis=-1))
        in_maps.append(m)
    return in_maps


def kernel(**inputs):
    in_maps = prep_inputs(inputs)
    nc, _ = build()
    res = run_bass_kernel_spmd(nc, in_maps, core_ids=list(range(8)))
    return np.stack([np.asarray(r["y"], dtype=np.float32) for r in res.results], axis=0)
```

```python
import numpy as np
import concourse.bass as bass
import concourse.mybir as mybir
from concourse.bass_utils import run_bass_kernel_spmd

F32 = mybir.dt.float32
BF16 = mybir.dt.bfloat16
AF = mybir.ActivationFunctionType
ALU = mybir.AluOpType

EPOCH = 20000
N_DMA_TL = 12


class Buf:
    __slots__ = ("name", "last_writer", "readers", "readers_d")

    def __init__(self, name):
        self.name = name
        self.last_writer = None
        self.readers = {}
        self.readers_d = []


class Op:
    __slots__ = ("eng", "fn", "reads", "writes", "dma", "deps", "signal", "sig", "id", "barrier")


class Prog:
    ENGS = ("pe", "act", "dve", "pool", "sp")

    def __init__(self, nc):
        self.nc = nc
        self.ops = []
        self.bufs = {}
        self._uid = 0

    def buf(self, *key):
        b = self.bufs.get(key)
        if b is None:
            b = Buf(key)
            self.bufs[key] = b
        return b

    def newbuf(self, tag="b"):
        self._uid += 1
        return self.buf(tag, self._uid)

    def op(self, eng, fn, reads=(), writes=(), dma=False):
        o = Op()
        o.eng = eng
        o.fn = fn
        o.reads = tuple(reads)
        o.writes = tuple(writes)
        o.dma = dma
        o.id = len(self.ops)
        o.signal = dma
        o.sig = None
        o.barrier = False
        o.deps = ()
        self.ops.append(o)
        return o

    def barrier(self):
        o = self.op(None, None)
        o.barrier = True
        return o

    def dma(self, eng, out, in_, reads=(), writes=(), **kw):
        return self.op(eng, lambda e: e.dma_start(out=out, in_=in_, **kw), reads, writes, dma=True)

    def finalize(self):
        nc = self.nc
        ops = self.ops
        last_on = {e: None for e in self.ENGS}
        for o in ops:
            if o.barrier:
                for e in self.ENGS:
                    if last_on[e] is not None:
                        ops[last_on[e]].signal = True
                for b in self.bufs.values():
                    b.last_writer = None
                    b.readers = {}
                    b.readers_d = []
                continue
            deps = set()
            for b in o.reads:
                if b.last_writer is not None:
                    deps.add(b.last_writer)
            for b in o.writes:
                if b.last_writer is not None:
                    deps.add(b.last_writer)
                deps.update(b.readers.values())
                deps.update(b.readers_d)
            for b in o.writes:
                b.last_writer = o.id
                b.readers = {}
                b.readers_d = []
            for b in o.reads:
                if b.last_writer != o.id:
                    if o.dma:
                        b.readers_d.append(o.id)
                    else:
                        b.readers[o.eng] = o.id
            deps.discard(o.id)
            fd = []
            for d in deps:
                p = ops[d]
                if (not p.dma) and (not o.dma) and p.eng == o.eng and o.eng == "pe":
                    continue
                fd.append(d)
            o.deps = fd
            for d in fd:
                ops[d].signal = True
            if not o.dma:
                last_on[o.eng] = o.id
        cnt = {e: 0 for e in self.ENGS}
        dma_rr = {e: 0 for e in self.ENGS}
        dma_uses = {}
        for o in ops:
            if o.barrier:
                continue
            if o.dma:
                k = dma_rr[o.eng] % N_DMA_TL
                dma_rr[o.eng] += 1
                tl = ("dma", o.eng, k)
                dma_uses[tl] = dma_uses.get(tl, 0) + 1
                o.sig = (tl, dma_uses[tl])
            elif o.signal:
                cnt[o.eng] += 1
                o.sig = (("eng", o.eng), cnt[o.eng])
        sems = {}
        for e in self.ENGS:
            n_ep = (cnt[e] + EPOCH - 1) // EPOCH
            for ep in range(max(n_ep, 1)):
                sems[(("eng", e), ep)] = nc.alloc_semaphore(f"s_{e}_{ep}")
        for tl in dma_uses:
            sems[(tl, 0)] = nc.alloc_semaphore(f"s_dma_{tl[1]}_{tl[2]}")

        def sem_of(tl, v):
            if tl[0] == "eng":
                ep = (v - 1) // EPOCH
                return sems[(tl, ep)], v - ep * EPOCH
            return sems[(tl, 0)], v * 16

        seen = {e: {} for e in self.ENGS}
        pending = {e: None for e in self.ENGS}
        cur = {}
        streams = {e: [] for e in self.ENGS}
        n_waits = 0
        for o in ops:
            if o.barrier:
                snap = dict(cur)
                for e in self.ENGS:
                    if pending[e] is None:
                        pending[e] = dict(snap)
                    else:
                        pending[e].update(snap)
                continue
            E = o.eng
            need = {}
            if pending[E] is not None:
                need.update(pending[E])
                pending[E] = None
            for d in o.deps:
                tl, v = ops[d].sig
                if need.get(tl, 0) < v:
                    need[tl] = v
            if o.dma:
                tl, v = o.sig
                if v > 1 and need.get(tl, 0) < v - 1:
                    need[tl] = v - 1
            waits = []
            for tl, v in need.items():
                if seen[E].get(tl, 0) < v:
                    seen[E][tl] = v
                    waits.append(sem_of(tl, v))
            n_waits += len(waits)
            inc = None
            if o.sig is not None:
                tl, v = o.sig
                cur[tl] = v
                s, _ = sem_of(tl, v)
                inc = (s, 16 if tl[0] == "dma" else 1)
            streams[E].append((waits, o.fn, inc))
        fin = []
        for tl, v in cur.items():
            fin.append(sem_of(tl, v))
        self.stats = dict(n_ops=len(ops), n_waits=n_waits, cnt=dict(cnt), n_dma=sum(dma_uses.values()),
                          n_sems=len(sems))
        engmap = {"pe": "tensor", "act": "scalar", "dve": "vector", "pool": "gpsimd", "sp": "sync"}
        with nc.Block() as block:
            for e in self.ENGS:
                lst = streams[e]
                is_sp = e == "sp"

                def body(eng, lst=lst, is_sp=is_sp):
                    for waits, fn, inc in lst:
                        for s, v in waits:
                            eng.wait_ge(s, v)
                        ins = fn(eng)
                        if inc is not None:
                            ins.then_inc(inc[0], inc[1])
                    if is_sp:
                        for s, v in fin:
                            eng.wait_ge(s, v)

                if lst or is_sp:
                    getattr(block, engmap[e])(body)
        return self.stats


class T:
    __slots__ = ("t", "b")

    def __init__(self, t, b):
        self.t = t
        self.b = b

    def __getitem__(self, k):
        return self.t[k]


class SBA:
    BASE = 16512
    END = 229376

    def __init__(self, nc, P):
        self.nc = nc
        self.P = P
        self.lo = self.BASE
        self.n = 0

    def alloc(self, shape, dtype):
        sz = 4 if dtype == F32 else 2
        n = sz
        for s in shape[1:]:
            n *= s
        n = (n + 63) // 64 * 64
        off = self.lo
        self.lo += n
        assert self.lo <= self.END, f"SBUF overflow {self.lo}"
        self.peak = max(getattr(self, "peak", 0), self.lo)
        self.n += 1
        t = self.nc.alloc_sbuf_tensor_at(f"sb{self.n}", list(shape), dtype, offset=off)
        return T(t, self.P.newbuf("sb"))

    def mark(self):
        return self.lo

    def release(self, m):
        self.lo = m


class Rot:
    def __init__(self, items):
        self.items = items
        self.i = 0

    def next(self):
        it = self.items[self.i % len(self.items)]
        self.i += 1
        return it


D = 1024
SEQ = 4096
CTX = 256
NH = 4
DH = 256
DFF = 2816
NE = 8
DFE = 3584
EPS = 1e-6
IN_COLS = 6672
POOL_W = (2, 4, 8, 16)
Q_OFF, K_OFF, V_OFF, O_OFF, IF_OFF = 0, 1024, 2048, 3072, 4096
POOL_OFF = 4112
GA_OFF = POOL_OFF + 512
GB_OFF = GA_OFF + 1024
NEG = -30000.0


def build(debug=None):
    nc = bass.Bass("TRN2", target_bir_lowering=False)
    P = Prog(nc)
    SB = SBA(nc, P)

    def din(name, shape, dt=F32):
        return nc.dram_tensor(name, list(shape), dt, kind="ExternalInput").ap()

    debug = debug or {}
    dbg_names = set(debug.get("names", ()))

    def dscr(name, shape, dt):
        kind = "ExternalOutput" if name in dbg_names else "Internal"
        return nc.dram_tensor(name, list(shape), dt, kind=kind).ap()

    x_in = din("x", [SEQ, D])
    ctx_in = din("ctx", [CTX, D])
    cc_in = din("cc", [128, 8, 2])
    w_ada = din("w_ada", [2, D, 6 * D])
    b_ada = din("b_ada", [2, 1, 6 * D])
    vecs_in = din("vecs", [2, 128, 28])
    convw_in = din("convw", [2, 128, 16, 3])
    w_in = din("w_in", [2, D, IN_COLS])
    w_if = din("w_if", [2, D, 16])
    b_if = din("b_if", [2, 1, 16])
    w_pool = din("w_pool", [2, 4, 128, 128])
    w_pa = din("w_pa", [2, D, D])
    w_pb = din("w_pb", [2, 512, D])
    w_out = din("w_out", [2, D, D])
    w_ffg = din("w_ff_gate", [1, D, DFF])
    w_ffu = din("w_ff_up", [1, D, DFF])
    w_ffd = din("w_ff_down", [1, DFF, D])
    w_router = din("w_router", [1, D, NE])
    w_eg = din("w_exp_gate", [1, NE, D, DFE])
    w_eu = din("w_exp_up", [1, NE, D, DFE])
    w_ed = din("w_exp_down", [1, NE, DFE, D])
    gfin_in = din("g_final_b", [128, D])
    cmat_in = din("cmat", [6, 128, 128])
    invc_lat = din("invc_lat", [4, 128, SEQ])
    invc_ctx = din("invc_ctx", [4, 128, CTX])
    y_out = nc.dram_tensor("y", [SEQ, D], F32, kind="ExternalOutput").ap()

    def mk_scr(sfx, NT):
        s = {}
        s["qT"] = dscr("qT" + sfx, [D, NT], BF16)
        s["kT"] = dscr("kT" + sfx, [D, NT], BF16)
        s["ktm"] = dscr("ktm" + sfx, [NT, D], BF16)
        s["vtm"] = dscr("vtm" + sfx, [NT, D], BF16)
        s["oT"] = dscr("oT" + sfx, [D, NT], BF16)
        s["gaT"] = dscr("gaT" + sfx, [D, NT], BF16)
        s["gbT"] = dscr("gbT" + sfx, [D, NT], BF16)
        s["pmwT"] = dscr("pmwT" + sfx, [512, NT], BF16)
        s["hf"] = dscr("hf" + sfx, [NT, D], F32)
        s["u2T"] = dscr("u2T" + sfx, [D, NT], BF16)
        s["h1"] = dscr("h1" + sfx, [NT, D], F32)
        s["h2"] = dscr("h2" + sfx, [NT, D], F32)
        return s

    scr_x = mk_scr("_x", SEQ)
    scr_c = mk_scr("_c", CTX)
    B = P.buf

    banks = []
    for i in range(8):
        banks.append(T(nc.alloc_psum_tensor(f"ps{i}", [128, 512], F32), B("bank", i)))
    bankrot = Rot(banks)

    def rb(*ts):
        return [t.b for t in ts]

    def ACT(out, in_, func, reads, writes, **kw):
        P.op("act", lambda e: e.activation(out=out, in_=in_, func=func, **kw), reads, writes)

    def TS(eng, out, in0, s1, s2, op0, op1, reads, writes):
        if op1 is None:
            P.op(eng, lambda e: e.tensor_scalar(out=out, in0=in0, scalar1=s1, scalar2=None, op0=op0), reads, writes)
        else:
            P.op(eng, lambda e: e.tensor_scalar(out=out, in0=in0, scalar1=s1, scalar2=s2, op0=op0, op1=op1), reads, writes)

    def TT_(eng, out, in0, in1, op, reads, writes):
        P.op(eng, lambda e: e.tensor_tensor(out=out, in0=in0, in1=in1, op=op), reads, writes)

    def STT(eng, out, in0, scalar, in1, op0, op1, reads, writes):
        P.op(eng, lambda e: e.scalar_tensor_tensor(out=out, in0=in0, scalar=scalar, in1=in1, op0=op0, op1=op1), reads, writes)

    def CP(eng, out, in_, reads, writes):
        if eng == "act":
            P.op(eng, lambda e: e.activation(out=out, in_=in_, func=AF.Identity), reads, writes)
        else:
            P.op(eng, lambda e: e.tensor_copy(out=out, in_=in_), reads, writes)

    def MS(eng, ap, val, writes):
        P.op(eng, lambda e: e.memset(ap, val), (), writes)

    def MM(bank, out, lhsT, rhs, start, stop, reads):
        P.op("pe", lambda e: e.matmul(out, lhsT=lhsT, rhs=rhs, start=start, stop=stop), reads, [bank.b])

    def TR(bank, out, in_, ident, reads):
        P.op("pe", lambda e: e.transpose(out=out, in_=in_, identity=ident), reads, [bank.b])

    def LD(dst_ap, src_ap, tile, q="sp", reads=(), **kw):
        P.dma(q, dst_ap, src_ap, reads=reads, writes=[tile.b], **kw)

    def ST(dst_ap, src_ap, tile, dbuf, q="sp"):
        P.dma(q, dst_ap, src_ap, reads=[tile.b], writes=[dbuf])

    cm = SB.alloc([128, 6, 128], F32)
    LD(cm[:], cmat_in.rearrange("c p n -> p c n"), cm)
    ident = cm[:, 0, :]
    tri = (cm[:, 1, :], cm[:, 2, :])
    negm = (cm[:, 3, :], cm[:, 4, :])
    ones = cm[:, 5, :]
    epsc = SB.alloc([128, 4], F32)
    MS("dve", epsc[:, 0:1], EPS, [epsc.b])
    MS("dve", epsc[:, 1:2], 1.0, [epsc.b])
    MS("dve", epsc[:, 2:3], float(-np.log(16.0)), [epsc.b])
    MS("dve", epsc[:, 3:4], 0.0, [epsc.b])
    scc = SB.alloc([128, 8, 2], F32)
    LD(scc[:], cc_in, scc)
    ACT(scc[:], scc[:], AF.Silu, [], [scc.b])
    m_persist = SB.mark()

    def adaln(l, want_ctx_gt):
        lay = {}
        modT = SB.alloc([128, 48, 2], F32)
        vecs = SB.alloc([128, 28], F32)
        LD(vecs[:], vecs_in[l], vecs)
        convw = SB.alloc([128, 16, 3], F32)
        LD(convw[:], convw_in[l], convw)
        s1 = SB.alloc([128, 8, 2], F32)
        s2 = SB.alloc([128, 8, 2], F32)
        gtb = {}
        for nm in (("gt1x", "gt2x", "gt1c", "gt2c") if want_ctx_gt else ("gt1x", "gt2x")):
            gtb[nm] = SB.alloc([128, D], F32)
        m0 = SB.mark()
        brow = SB.alloc([1, 6 * D], F32)
        LD(brow[:], b_ada[l], brow)
        rep = SB.alloc([128, 2, 8, 128], F32)
        for v in range(2):
            for k in range(8):
                TS("dve", rep[:, v, k, :], ones, scc[:, k, v:v + 1], None, ALU.mult, None, [cm.b, scc.b], [rep.b])
        wblk = [SB.alloc([128, 8, 512], F32) for _ in range(2)]
        for j in range(12):
            wb = wblk[j % 2]
            LD(wb[:], w_ada[l, :, j * 512:(j + 1) * 512].rearrange("(k p) n -> p k n", p=128), wb)
            bank = bankrot.next()
            for c in range(4):
                for k in range(8):
                    MM(bank, bank[:, c * 2:c * 2 + 2], wb[:, k, c * 128:(c + 1) * 128], scc[:, k, :], k == 0, False,
                       [wb.b, scc.b])
                MM(bank, bank[:, c * 2:c * 2 + 2], brow[0:1, j * 512 + c * 128:j * 512 + (c + 1) * 128], ones[0:1, 0:2],
                   False, True, [brow.b, cm.b])
            CP("dve", modT[:, j * 4:(j + 1) * 4, :], bank[:, 0:8].rearrange("p (c v) -> p c v", v=2), [], [bank.b, modT.b])
            which = {4: ("gt1", 0), 5: ("gt1", 1), 10: ("gt2", 0), 11: ("gt2", 1)}.get(j)
            if which is not None:
                for v, sfx in ((0, "x"), (1, "c")):
                    nm = which[0] + sfx
                    if nm not in gtb:
                        continue
                    bank = bankrot.next()
                    for k in range(8):
                        MM(bank, bank[:, :], rep[:, v, k, :], wb[:, k, :], k == 0, False, [rep.b, wb.b])
                    MM(bank, bank[:, :], ones[0:1, :], brow[0:1, j * 512:(j + 1) * 512], False, True, [cm.b, brow.b])
                    CP("act", gtb[nm][:, which[1] * 512:(which[1] + 1) * 512], bank[:, :], [], [bank.b, gtb[nm].b])
        for v in range(2):
            TS("dve", s1[:, :, v], modT[:, 8:16, v], 1.0, None, ALU.add, None, [modT.b], [s1.b])
            TT_("dve", s1[:, :, v], s1[:, :, v], vecs[:, 0:8], ALU.mult, [s1.b, vecs.b], [s1.b])
            TS("dve", s2[:, :, v], modT[:, 32:40, v], 1.0, None, ALU.add, None, [modT.b], [s2.b])
            TT_("dve", s2[:, :, v], s2[:, :, v], vecs[:, 16:24], ALU.mult, [s2.b, vecs.b], [s2.b])
        P.barrier()
        SB.release(m0)
        lay.update(modT=modT, vecs=vecs, convw=convw, s1=s1, s2=s2, gtb=gtb)
        return lay

    def norm_transpose(src_tile, reads_src, sc_ap, bi_ap, out_fn, tmp):
        ss, xn = tmp
        ACT(xn[:], src_tile, AF.Square, reads_src, [xn.b, ss.b], accum_out=ss[:, 0:1])
        ACT(ss[:, 1:2], ss[:, 0:1], AF.Sqrt, [epsc.b], [ss.b], scale=1.0 / D, bias=epsc[:, 0:1])
        P.op("dve", lambda e: e.reciprocal(out=ss[:, 1:2], in_=ss[:, 1:2]), [], [ss.b])
        TS("dve", xn[:], src_tile, ss[:, 1:2], None, ALU.mult, None, list(reads_src) + [ss.b], [xn.b])
        for hf in range(debug.get("nhf", 2)):
            bank = bankrot.next()
            for i in range(4):
                k = hf * 4 + i
                TR(bank, bank[:, i * 128:(i + 1) * 128], xn[:, k * 128:(k + 1) * 128], ident, [xn.b, cm.b])
            for i in range(4):
                k = hf * 4 + i
                o_ap, o_bufs = out_fn(k)
                sc, scb = sc_ap(k)
                bi, bib = bi_ap(k)
                if i % 2 == 0:
                    ACT(o_ap, bank[:, i * 128:(i + 1) * 128], AF.Identity, [scb, bib], [bank.b] + list(o_bufs), scale=sc, bias=bi)
                else:
                    TS("dve", o_ap, bank[:, i * 128:(i + 1) * 128], sc, bi, ALU.mult, ALU.add, [scb, bib], [bank.b] + list(o_bufs))

    def phase_proj(l, lay, seq, h_src):
        NT = seq["NT"]
        TT = NT // 128
        var = seq["var"]
        scr = seq["scr"]
        TK = min(512, NT)
        nTK = NT // TK
        mA = SB.mark()
        uT = SB.alloc([128, 8, NT], BF16)
        gsb = seq["gsb"]
        m1 = SB.mark()
        xts = Rot([SB.alloc([128, D], F32) for _ in range(3)])
        tmps = Rot([(SB.alloc([128, 2], F32), SB.alloc([128, D], F32)) for _ in range(2)])
        for t in range(TT if NT == CTX else debug.get("a1_tiles", TT)):
            xt = xts.next()
            LD(xt[:], h_src[t * 128:(t + 1) * 128, :], xt, reads=[seq["hbuf"]])
            norm_transpose(xt[:], [xt.b], lambda k: (lay["s1"][:, k, var:var + 1], lay["s1"].b),
                           lambda k: (lay["modT"][:, k, var:var + 1], lay["modT"].b),
                           lambda k, t=t: (uT[:, k, t * 128:(t + 1) * 128], [uT.b]), tmps.next())
        P.barrier()
        SB.release(m1)
        if debug.get("sub") == "A1" and NT == SEQ:
            return
        wrot = Rot([SB.alloc([128, 8, 512], BF16) for _ in range(2)])

        def load_w(col0, ncols=512):
            wb = wrot.next()
            LD(wb[:, :, 0:ncols], w_in[l, :, col0:col0 + ncols].rearrange("(k p) n -> p k n", p=128), wb, q="pool")
            return wb

        def fm_chunk(wb, c, evac):
            for tk in range(nTK):
                bank = bankrot.next()
                for k in range(8):
                    MM(bank, bank[:, 0:TK], wb[:, k, c * 128:(c + 1) * 128], uT[:, k, tk * TK:(tk + 1) * TK], k == 0, k == 7,
                       [wb.b, uT.b])
                evac(bank, tk)

        m2 = SB.mark()
        zcs = Rot([SB.alloc([128, NT + 2], F32) for _ in range(2)])
        for z in zcs.items:
            MS("dve", z[:], 0.0, [z.b])
        caccs = Rot([SB.alloc([128, NT], F32) for _ in range(2)])
        qkbfs = Rot([SB.alloc([128, NT], BF16) for _ in range(1)])
        ktsts = Rot([SB.alloc([128, 4, 128], BF16) for _ in range(4)])
        convw = lay["convw"]
        def post_chunk(cg, zc):
            if True:
                cacc = caccs.next()
                TS("dve", cacc[:], zc[:, 1:NT + 1], convw[:, cg, 1:2], None, ALU.mult, None, [zc.b, convw.b], [cacc.b])
                STT("dve", cacc[:], zc[:, 0:NT], convw[:, cg, 0:1], cacc[:], ALU.mult, ALU.add, [zc.b, convw.b], [cacc.b])
                STT("dve", cacc[:], zc[:, 2:NT + 2], convw[:, cg, 2:3], cacc[:], ALU.mult, ALU.add, [zc.b, convw.b], [cacc.b])
                qb = qkbfs.next()
                if cg < 8:
                    ACT(qb[:], cacc[:], AF.Silu, [cacc.b], [qb.b])
                    ST(scr["qT"][cg * 128:(cg + 1) * 128, :], qb[:], qb, B("qT", var))
                else:
                    kc = cg - 8
                    ACT(cacc[:], cacc[:], AF.Silu, [], [cacc.b])
                    CP("dve", qb[:], cacc[:], [cacc.b], [qb.b])
                    ST(scr["kT"][kc * 128:(kc + 1) * 128, :], qb[:], qb, B("kT", var))
                    for t0 in range(0, TT, 4):
                        nt = min(4, TT - t0)
                        bank = bankrot.next()
                        for i in range(nt):
                            TR(bank, bank[:, i * 128:(i + 1) * 128], cacc[:, (t0 + i) * 128:(t0 + i + 1) * 128], ident,
                               [cacc.b, cm.b])
                        kst = ktsts.next()
                        CP("act", kst[:, 0:nt, :], bank[:, 0:nt * 128].rearrange("p (t d) -> p t d", d=128), [],
                           [bank.b, kst.b])
                        ST(scr["ktm"][t0 * 128:(t0 + nt) * 128, kc * 128:(kc + 1) * 128].rearrange("(t p) d -> p t d", p=128),
                           kst[:, 0:nt, :], kst, B("ktm", var))

        prev = None
        for blk in range(4):
            wb = load_w(blk * 512)
            for c in range(4):
                cg = blk * 4 + c
                zc = zcs.next()
                fm_chunk(wb, c, lambda bank, tk, zc=zc: CP("act", zc[:, 1 + tk * TK:1 + (tk + 1) * TK], bank[:, 0:TK], [],
                                                          [bank.b, zc.b]))
                if prev is not None:
                    post_chunk(*prev)
                prev = (cg, zc)
        post_chunk(*prev)
        P.barrier()
        SB.release(m2)
        if debug.get("sub") == "A2a" and NT == SEQ:
            return
        m3 = SB.mark()
        wif = SB.alloc([128, 8, 16], BF16)
        LD(wif[:], w_if[l].rearrange("(k p) n -> p k n", p=128), wif, q="pool")
        bif = SB.alloc([1, 16], F32)
        LD(bif[:], b_if[l], bif)
        vsts = Rot([SB.alloc([128, 512], BF16) for _ in range(3)])
        for blk in range(2):
            wb = load_w(V_OFF + blk * 512)
            for t in range(TT):
                bank = bankrot.next()
                for k in range(8):
                    MM(bank, bank[:, :], uT[:, k, t * 128:(t + 1) * 128], wb[:, k, :], k == 0, k == 7, [uT.b, wb.b])
                vst = vsts.next()
                CP("act" if t % 2 == 0 else "dve", vst[:], bank[:, :], [], [bank.b, vst.b])
                ST(scr["vtm"][t * 128:(t + 1) * 128, blk * 512:(blk + 1) * 512], vst[:], vst, B("vtm", var))
        for t0 in range(0, TT, 16):
            nt = min(16, TT - t0)
            bank = bankrot.next()
            for i in range(nt):
                t = t0 + i
                for k in range(8):
                    MM(bank, bank[:, i * 16:(i + 1) * 16], uT[:, k, t * 128:(t + 1) * 128], wif[:, k, :], k == 0, False,
                       [uT.b, wif.b])
                MM(bank, bank[:, i * 16:(i + 1) * 16], ones[0:1, :], bif[0:1, :], False, True, [cm.b, bif.b])
            CP("dve", gsb[:, t0:t0 + nt, :], bank[:, 0:nt * 16].rearrange("p (t g) -> p t g", g=16), [], [bank.b, gsb.b])
        sgs = Rot([SB.alloc([128, TK], BF16) for _ in range(3)])

        def sig_block(col0, dst, dname):
            for blk in range(2):
                wb = load_w(col0 + blk * 512)
                for c in range(4):
                    cg = blk * 4 + c

                    def ev(bank, tk, cg=cg):
                        sg = sgs.next()
                        ACT(sg[:], bank[:, 0:TK], AF.Sigmoid, [], [bank.b, sg.b])
                        ST(dst[cg * 128:(cg + 1) * 128, tk * TK:(tk + 1) * TK], sg[:], sg, B(dname, var))
                    fm_chunk(wb, c, ev)

        if seq["with_out"]:
            sig_block(O_OFF, scr["oT"], "oT")
            sig_block(GA_OFF, scr["gaT"], "gaT")
            sig_block(GB_OFF, scr["gbT"], "gbT")
        P.barrier()
        SB.release(m3)
        if debug.get("sub") == "A2b" and NT == SEQ:
            return
        if seq["with_out"]:
            m4 = SB.mark()
            wpl = SB.alloc([128, 4, 128], BF16)
            LD(wpl[:], w_pool[l].rearrange("g c e -> c g e"), wpl, q="pool")
            wb = load_w(POOL_OFF)
            pz = SB.alloc([128, NT], F32)
            a1 = SB.alloc([128, NT], F32)
            a2 = SB.alloc([128, NT], F32)
            ivc = SB.alloc([128, NT], F32)
            pmb = SB.alloc([128, NT], BF16)
            sgs = Rot([SB.alloc([128, TK], BF16) for _ in range(3)])
            invc_src = invc_ctx if NT == CTX else invc_lat
            for g in range(4):
                w = POOL_W[g]
                LD(ivc[:], invc_src[g], ivc)
                fm_chunk(wb, g, lambda bank, tk: CP("act", pz[:, tk * TK:(tk + 1) * TK], bank[:, 0:TK], [], [bank.b, pz.b]))
                if NT == CTX:
                    CP("dve", a2[:], pz[:], [pz.b], [a2.b])
                    for o in range(-(w // 2), w // 2):
                        if o == 0:
                            continue
                        lo, hi = max(0, -o), min(NT, NT - o)
                        TT_("dve", a2[:, lo:hi], a2[:, lo:hi], pz[:, lo + o:hi + o], ALU.add, [pz.b], [a2.b])
                else:
                    G = 64
                    pz3 = pz[:].rearrange("p (r x) -> p r x", x=G)
                    a13 = a1[:].rearrange("p (r x) -> p r x", x=G)
                    CP("dve", a1[:], pz[:], [pz.b], [a1.b])
                    for o in range(-(w // 2), w // 2):
                        if o == 0:
                            continue
                        lo, hi = max(0, -o), min(G, G - o)
                        TT_("dve", a13[:, :, lo:hi], a13[:, :, lo:hi], pz3[:, :, lo + o:hi + o], ALU.add, [pz.b], [a1.b])
                    CP("dve", a2[:], a1[:], [a1.b], [a2.b])
                    for o in range(-(w // 2), w // 2):
                        if o == 0:
                            continue
                        lo, hi = max(0, -o), min(G, G - o)
                        TT_("dve", a2[:, lo * G:hi * G], a2[:, lo * G:hi * G], a1[:, (lo + o) * G:(hi + o) * G], ALU.add,
                            [a1.b], [a2.b])
                TT_("dve", a2[:], a2[:], ivc[:], ALU.mult, [ivc.b], [a2.b])
                TT_("dve", pmb[:], a2[:], pz[:], ALU.subtract, [a2.b, pz.b], [pmb.b])
                for tk in range(nTK):
                    bank = bankrot.next()
                    MM(bank, bank[:, 0:TK], wpl[:, g, :], pmb[:, tk * TK:(tk + 1) * TK], True, True, [wpl.b, pmb.b])
                    sg = sgs.next()
                    ACT(sg[:], bank[:, 0:TK], AF.Identity, [lay["vecs"].b], [bank.b, sg.b], scale=lay["vecs"][:, 24 + g:25 + g])
                    ST(scr["pmwT"][g * 128:(g + 1) * 128, tk * TK:(tk + 1) * TK], sg[:], sg, B("pmwT", var))
            P.barrier()
            SB.release(m4)
        P.barrier()
        SB.release(mA)

    def gate_prep(seq):
        TT = seq["NT"] // 128
        gsb = seq["gsb"]
        gd = seq["gd"]
        lf, bc, a_s, wk, ebl = gd["lf"], gd["bc"], gd["a_s"], gd["wk"], gd["ebl"]
        gf = gsb[:, :, 8:16]
        ACT(a_s[:], gf, AF.Abs, [gsb.b], [a_s.b])
        ACT(a_s[:], a_s[:], AF.Exp, [], [a_s.b], scale=-1.0)
        ACT(a_s[:], a_s[:], AF.Ln, [epsc.b], [a_s.b], bias=epsc[:, 1:2])
        TS("dve", lf[:], gf, 0.0, None, ALU.min, None, [gsb.b], [lf.b])
        TT_("dve", lf[:], lf[:], a_s[:], ALU.subtract, [a_s.b], [lf.b])
        for d in range(2):
            bank = bankrot.next()
            for t in range(TT):
                MM(bank, bank[:, t * 4:(t + 1) * 4], tri[d], lf[:, t, d * 4:(d + 1) * 4], True, True, [cm.b, lf.b])
            CP("dve", bc[:, :, d * 4:(d + 1) * 4], bank[:, 0:TT * 4].rearrange("p (t h) -> p t h", h=4), [], [bank.b, bc.b])
        bank = bankrot.next()
        MM(bank, bank[:, 0:TT * 8], ones, lf[:].rearrange("p t g -> p (t g)"), True, True, [cm.b, lf.b])
        CP("dve", ebl[:].rearrange("p t g -> p (t g)"), bank[:, 0:TT * 8], [], [bank.b, ebl.b])
        TT_("dve", a_s[:], gsb[:, :, 0:8], bc[:], ALU.subtract, [gsb.b, bc.b], [a_s.b])
        TT_("dve", wk[:], a_s[:], ebl[:], ALU.add, [a_s.b, ebl.b], [wk.b])
        ACT(wk[:], wk[:], AF.Exp, [epsc.b], [wk.b], bias=epsc[:, 2:3])
        ACT(ebl[:], ebl[:], AF.Exp, [wk.b], [ebl.b])

    def scan_chunk(seq, t, d, data, st, work, with_out, htile):
        gsb, gd = seq["gsb"], seq["gd"]
        lf, bc, wk, ebl = gd["lf"], gd["bc"], gd["wk"], gd["ebl"]
        qTt, kTt, ktmt, vt = data
        Cf, Cb = st
        H = range(NH)
        W = [dict() for _ in H]
        for h in H:
            dh = d * 4 + h
            w = W[h]
            w["dh"] = dh
            w["vw"] = work["vw"].next()
            ACT(w["vw"][:], vt[:, h, :], AF.Identity, [vt.b, wk.b], [w["vw"].b], scale=wk[:, t, dh:dh + 1])
            if with_out:
                for nm in ("lfrep", "arg", "dec", "ebr", "sdT", "qs", "den"):
                    w[nm] = work[nm].next()
                ACT(w["lfrep"][:], ones, AF.Identity, [cm.b, lf.b], [w["lfrep"].b], scale=lf[:, t, dh:dh + 1])
        for h in H:
            W[h]["bx"] = bankrot.next()
        for h in H:
            W[h]["bw"] = bankrot.next()
        for h in H:
            w = W[h]
            dh = w["dh"]
            bx = w["bx"]
            if with_out:
                MM(bx, bx[:, 0:128], w["lfrep"][:], tri[d], True, True, [w["lfrep"].b, cm.b])
                for dc in range(2):
                    MM(bx, bx[:, 128:256], kTt[:, h * 2 + dc, :], qTt[:, h * 2 + dc, :], dc == 0, dc == 1, [kTt.b, qTt.b])
            bw = w["bw"]
            vw = w["vw"]
            for dc in range(2):
                MM(bw, bw[:, dc * 256:(dc + 1) * 256], ktmt[:, h * 256 + dc * 128:h * 256 + (dc + 1) * 128], vw[:, 0:256], True, True,
                   [ktmt.b, vw.b])
            for dc in range(2):
                MM(bx, bx[:, 256 + dc:257 + dc], ktmt[:, h * 256 + dc * 128:h * 256 + (dc + 1) * 128], vw[:, 256:257], True, True,
                   [ktmt.b, vw.b])
        def n_update(h):
            w = W[h]
            dh, bx = w["dh"], w["bx"]
            STT("dve", Cf[:, dh, :, 256:257], Cf[:, dh, :, 256:257], ebl[:, t, dh:dh + 1],
                bx[:, 256:258].rearrange("p (c e) -> p c e", c=2), ALU.mult, ALU.add, [ebl.b], [bx.b, B("Cf", dh)])

        if with_out:
            for h in H:
                w = W[h]
                dh, bx = w["dh"], w["bx"]
                STT("dve", w["arg"][:], bx[:, 0:128], bc[:, t, dh:dh + 1], negm[d], ALU.subtract, ALU.add, [bc.b, cm.b],
                    [bx.b, w["arg"].b])
                ACT(w["ebr"][:], bx[:, 0:128], AF.Exp, [], [bx.b, w["ebr"].b])
            for h in H:
                w = W[h]
                ACT(w["dec"][:], w["arg"][:], AF.Exp, [w["arg"].b, gsb.b], [w["dec"].b], bias=gsb[:, t, w["dh"]:w["dh"] + 1])
            for h in H:
                w = W[h]
                bx = w["bx"]
                STT("dve", w["sdT"][:], bx[:, 128:256], 1.0 / 16.0, w["dec"][:], ALU.mult, ALU.mult, [w["dec"].b], [bx.b, w["sdT"].b])
                n_update(h)
                for dc in range(2):
                    TT_("dve", w["qs"][:, dc, :], qTt[:, h * 2 + dc, :], w["ebr"][:], ALU.mult, [qTt.b, w["ebr"].b], [w["qs"].b])
        for h in H:
            w = W[h]
            dh, bw = w["dh"], w["bw"]
            STT("dve", Cf[:, dh, :, 0:256], Cf[:, dh, :, 0:256], ebl[:, t, dh:dh + 1], bw[:, :].rearrange("p (c e) -> p c e", c=2),
                ALU.mult, ALU.add, [ebl.b], [bw.b, B("Cf", dh)])
        if not with_out:
            for h in H:
                n_update(h)
        if with_out:
            for h in H:
                w = W[h]
                dh = w["dh"]
                bz = w["bz"] = bankrot.next()
                for dc in range(2):
                    MM(bz, bz[:, 0:257], w["qs"][:, dc, :], Cb[:, dh, dc, :], dc == 0, False, [w["qs"].b, B("Cb", dh)])
                MM(bz, bz[:, 0:257], w["sdT"][:], vt[:, h, :], False, True, [w["sdT"].b, vt.b])
        if with_out:
            for h in H:
                w = W[h]
                ACT(w["den"][:, 0:1], w["bz"][:, 256:257], AF.Abs, [], [w["bz"].b, w["den"].b])
        for h in H:
            dh = W[h]["dh"]
            CP("act", Cb[:, dh, :, :], Cf[:, dh, :, :], [B("Cf", dh)], [B("Cb", dh)])
        if with_out:
            for h in H:
                den = W[h]["den"]
                TS("dve", den[:, 0:1], den[:, 0:1], 1.0, None, ALU.max, None, [], [den.b])
            for h in H:
                den = W[h]["den"]
                P.op("dve", lambda e, den=den: e.reciprocal(out=den[:, 0:1], in_=den[:, 0:1]), [], [den.b])
            for h in H:
                w = W[h]
                TS("dve", htile[:, h * 256:(h + 1) * 256], w["bz"][:, 0:256], w["den"][:, 0:1], None, ALU.mult, None, [w["den"].b],
                   [w["bz"].b, htile.b])

    def phase_scan(l, lay, seq, h_src, last_layer):
        NT = seq["NT"]
        TT = NT // 128
        var = seq["var"]
        scr = seq["scr"]
        with_out = seq["with_out"]
        Cf, Cb = seq["Cf"], seq["Cb"]
        gate_prep(seq)
        mB = SB.mark()
        work = {
            "lfrep": Rot([SB.alloc([128, 128], F32) for _ in range(4)]),
            "arg": Rot([SB.alloc([128, 128], F32) for _ in range(4)]),
            "dec": Rot([SB.alloc([128, 128], F32) for _ in range(4)]),
            "ebr": Rot([SB.alloc([128, 128], BF16) for _ in range(4)]),
            "sdT": Rot([SB.alloc([128, 128], BF16) for _ in range(4)]),
            "qs": Rot([SB.alloc([128, 2, 128], BF16) for _ in range(4)]),
            "den": Rot([SB.alloc([128, 2], F32) for _ in range(4)]),
            "vw": Rot([SB.alloc([128, 257], BF16) for _ in range(4)]),
        }
        dq = Rot([SB.alloc([128, 8, 128], BF16) for _ in range(2)])
        dk = Rot([SB.alloc([128, 8, 128], BF16) for _ in range(2)])
        dkt = Rot([SB.alloc([128, D], BF16) for _ in range(2)])
        dv = Rot([SB.alloc([128, 4, 257], BF16) for _ in range(2)])
        for v_ in dv.items:
            MS("dve", v_[:], 1.0, [v_.b])

        def load_chunk(t):
            qTt, kTt, ktmt, vt = dq.next(), dk.next(), dkt.next(), dv.next()
            if with_out:
                LD(qTt[:], scr["qT"][:, t * 128:(t + 1) * 128].rearrange("(c p) t -> p c t", p=128), qTt, reads=[B("qT", var)])
                LD(kTt[:], scr["kT"][:, t * 128:(t + 1) * 128].rearrange("(c p) t -> p c t", p=128), kTt, reads=[B("kT", var)])
            LD(ktmt[:], scr["ktm"][t * 128:(t + 1) * 128, :], ktmt, reads=[B("ktm", var)])
            LD(vt[:, :, 0:256], scr["vtm"][t * 128:(t + 1) * 128, :].rearrange("p (h e) -> p h e", e=256), vt,
               reads=[B("vtm", var)])
            return (qTt, kTt, ktmt, vt)

        hfs = Rot([SB.alloc([128, D], F32) for _ in range(2)])
        nxt = load_chunk(0)
        for t in range(TT):
            data = nxt
            if t + 1 < TT:
                nxt = load_chunk(t + 1)
            hft = hfs.next()
            scan_chunk(seq, t, 0, data, (Cf, Cb), work, with_out, hft)
            if with_out:
                ST(scr["hf"][t * 128:(t + 1) * 128, :], hft[:], hft, B("hf", var))
        if not with_out:
            nxt = load_chunk(TT - 1)
            for t in range(TT - 1, -1, -1):
                data = nxt
                if t > 0:
                    nxt = load_chunk(t - 1)
                scan_chunk(seq, t, 1, data, (Cf, Cb), work, False, None)
            P.barrier()
            SB.release(mB)
            return
        wpa = SB.alloc([128, 8, D], BF16)
        LD(wpa[:], w_pa[l].rearrange("(k p) n -> p k n", p=128), wpa, q="pool")
        wpb = SB.alloc([128, 4, D], BF16)
        LD(wpb[:], w_pb[l].rearrange("(k p) n -> p k n", p=128), wpb, q="pool")
        wo = SB.alloc([128, 8, D], BF16)
        LD(wo[:], w_out[l].rearrange("(k p) n -> p k n", p=128), wo, q="pool")
        GT = min(4, TT)
        GN = GT * 128
        hbs = Rot([SB.alloc([128, D], F32) for _ in range(1)])
        stats = SB.alloc([128, 4, 6], F32)
        mv = SB.alloc([128, 4, 2], F32)
        hmT = SB.alloc([128, 8, GN], BF16)
        oTt = SB.alloc([128, 8, GN], BF16)
        gaTt = SB.alloc([128, 8, GN], BF16)
        gbTt = SB.alloc([128, 8, GN], BF16)
        pmTt = SB.alloc([128, 4, GN], BF16)
        mT = SB.alloc([128, 8, GN], BF16)
        t1s = Rot([SB.alloc([128, GN], F32) for _ in range(2)])
        t2s = Rot([SB.alloc([128, GN], F32) for _ in range(1)])
        hos = Rot([SB.alloc([128, D], F32) for _ in range(1)])
        hns = Rot([SB.alloc([128, D], F32) for _ in range(2)])
        tmps = Rot([(SB.alloc([128, 2], F32), SB.alloc([128, D], F32)) for _ in range(2)])
        u2s = Rot([SB.alloc([128, 8, 128], BF16) for _ in range(2)])
        u2f = SB.alloc([128, 8, 128], F32) if last_layer else None
        if last_layer:
            wr = SB.alloc([128, 8, NE], F32)
            LD(wr[:], w_router[0].rearrange("(k p) n -> p k n", p=128), wr)
            mx = SB.alloc([128, 8], F32)
            lg = SB.alloc([128, 8], F32)
            msk = SB.alloc([128, 8], F32)
            sm = SB.alloc([128, 4], F32)
        gt1 = lay["gtb"]["gt1" + ("x" if var == 0 else "c")]
        vecs = lay["vecs"]
        def load_chunk_b(t):
            dat = load_chunk(t)
            hf_ = hfs.next()
            LD(hf_[:], scr["hf"][t * 128:(t + 1) * 128, :], hf_, reads=[B("hf", var)])
            return dat, hf_

        def tail_tile(g, ti):
            t = g * GT + ti
            ho, hn = hos.next(), hns.next()
            LD(ho[:], h_src[t * 128:(t + 1) * 128, :], ho, reads=[seq["hbuf"]])
            for hf in range(2):
                bank = bankrot.next()
                for k in range(8):
                    MM(bank, bank[:, :], mT[:, k, ti * 128:(ti + 1) * 128], wo[:, k, hf * 512:(hf + 1) * 512], k == 0, k == 7,
                       [mT.b, wo.b])
                TT_("dve", hn[:, hf * 512:(hf + 1) * 512], bank[:, :], gt1[:, hf * 512:(hf + 1) * 512], ALU.mult, [gt1.b],
                    [bank.b, hn.b])
            TT_("dve", hn[:], hn[:], ho[:], ALU.add, [ho.b], [hn.b])
            ST(scr["h1"][t * 128:(t + 1) * 128, :], hn[:], hn, B("h1", var), q="pool")
            u2 = u2s.next()
            if last_layer:
                norm_transpose(hn[:], [hn.b], lambda k: (lay["s2"][:, k, var:var + 1], lay["s2"].b),
                               lambda k: (lay["modT"][:, 24 + k, var:var + 1], lay["modT"].b),
                               lambda k: (u2f[:, k, :], [u2f.b]), tmps.next())
                CP("act", u2[:], u2f[:], [u2f.b], [u2.b])
                bank = bankrot.next()
                for k in range(8):
                    MM(bank, bank[:, 0:NE], u2f[:, k, :], wr[:, k, :], k == 0, k == 7, [u2f.b, wr.b])
                CP("dve", lg[:], bank[:, 0:NE], [], [bank.b, lg.b])
                P.op("dve", lambda e: e.max(out=mx[:], in_=lg[:]), [lg.b], [mx.b])
                TS("dve", msk[:], lg[:], mx[:, 1:2], None, ALU.is_ge, None, [lg.b, mx.b], [msk.b])
                TS("dve", sm[:, 0:1], mx[:, 0:1], -1.0, None, ALU.mult, None, [mx.b], [sm.b])
                ACT(lg[:], lg[:], AF.Exp, [sm.b], [lg.b], bias=sm[:, 0:1])
                ACT(sm[:, 1:2], mx[:, 1:2], AF.Exp, [sm.b, mx.b], [sm.b], bias=sm[:, 0:1])
                TS("dve", sm[:, 1:2], sm[:, 1:2], 1.0, None, ALU.add, None, [], [sm.b])
                P.op("dve", lambda e: e.reciprocal(out=sm[:, 1:2], in_=sm[:, 1:2]), [], [sm.b])
                TT_("dve", lg[:], lg[:], msk[:], ALU.mult, [msk.b], [lg.b])
                TS("dve", seq["moeg"][:, t, :], lg[:], sm[:, 1:2], None, ALU.mult, None, [lg.b, sm.b], [seq["moeg"].b])
            else:
                norm_transpose(hn[:], [hn.b], lambda k: (lay["s2"][:, k, var:var + 1], lay["s2"].b),
                               lambda k: (lay["modT"][:, 24 + k, var:var + 1], lay["modT"].b),
                               lambda k: (u2[:, k, :], [u2.b]), tmps.next())
            ST(scr["u2T"][:, t * 128:(t + 1) * 128].rearrange("(c p) t -> p c t", p=128), u2[:], u2, B("u2T", var), q="pool")

        nxt = load_chunk_b(TT - 1)
        pending = []
        for g in range(TT // GT - 1, -1, -1):
            T0 = g * GN
            LD(oTt[:], scr["oT"][:, T0:T0 + GN].rearrange("(c p) t -> p c t", p=128), oTt, q="pool", reads=[B("oT", var)])
            LD(gaTt[:], scr["gaT"][:, T0:T0 + GN].rearrange("(c p) t -> p c t", p=128), gaTt, q="pool", reads=[B("gaT", var)])
            LD(gbTt[:], scr["gbT"][:, T0:T0 + GN].rearrange("(c p) t -> p c t", p=128), gbTt, q="pool", reads=[B("gbT", var)])
            LD(pmTt[:], scr["pmwT"][:, T0:T0 + GN].rearrange("(c p) t -> p c t", p=128), pmTt, q="pool", reads=[B("pmwT", var)])
            for ti in range(GT - 1, -1, -1):
                t = g * GT + ti
                data, hft = nxt
                if t > 0:
                    nxt = load_chunk_b(t - 1)
                hbt = hbs.next()
                scan_chunk(seq, t, 1, data, (Cf, Cb), work, True, hbt)
                TT_("dve", hbt[:], hbt[:], hft[:], ALU.add, [hft.b], [hbt.b])
                for h in range(NH):
                    P.op("dve", lambda e, h=h, hbt=hbt: e.bn_stats(out=stats[:, h, :], in_=hbt[:, h * 256:(h + 1) * 256]),
                         [hbt.b], [stats.b])
                    P.op("dve", lambda e, h=h: e.bn_aggr(out=mv[:, h, :], in_=stats[:, h, :]), [stats.b], [mv.b])
                ACT(mv[:, :, 1], mv[:, :, 1], AF.Sqrt, [epsc.b], [mv.b], bias=epsc[:, 0:1])
                P.op("dve", lambda e: e.reciprocal(out=mv[:, :, 1], in_=mv[:, :, 1]), [], [mv.b])
                for h in range(NH):
                    TS("dve", hbt[:, h * 256:(h + 1) * 256], hbt[:, h * 256:(h + 1) * 256], mv[:, h, 0:1], mv[:, h, 1:2],
                       ALU.subtract, ALU.mult, [mv.b], [hbt.b])
                for hf in range(2):
                    bank = bankrot.next()
                    for i in range(4):
                        k = hf * 4 + i
                        TR(bank, bank[:, i * 128:(i + 1) * 128], hbt[:, k * 128:(k + 1) * 128], ident, [hbt.b, cm.b])
                    for i in range(4):
                        k = hf * 4 + i
                        STT("dve", hmT[:, k, ti * 128:(ti + 1) * 128], bank[:, i * 128:(i + 1) * 128], vecs[:, 8 + k:9 + k],
                            oTt[:, k, ti * 128:(ti + 1) * 128], ALU.mult, ALU.mult, [vecs.b, oTt.b], [bank.b, hmT.b])
                if pending:
                    tail_tile(*pending.pop(0))
            while pending:
                tail_tile(*pending.pop(0))
            for f in range(8):
                ba = bankrot.next()
                for k in range(8):
                    MM(ba, ba[:, 0:GN], wpa[:, k, f * 128:(f + 1) * 128], hmT[:, k, :], k == 0, k == 7, [wpa.b, hmT.b])
                bb = bankrot.next()
                for k in range(4):
                    MM(bb, bb[:, 0:GN], wpb[:, k, f * 128:(f + 1) * 128], pmTt[:, k, :], k == 0, k == 3, [wpb.b, pmTt.b])
                t1, t2 = t1s.next(), t2s.next()
                TT_("dve", t1[:], ba[:, 0:GN], gaTt[:, f, :], ALU.mult, [gaTt.b], [ba.b, t1.b])
                TT_("dve", t2[:], bb[:, 0:GN], gbTt[:, f, :], ALU.mult, [gbTt.b], [bb.b, t2.b])
                TT_("dve", mT[:, f, :], t1[:], t2[:], ALU.add, [t1.b, t2.b], [mT.b])
            pending = [(g, ti) for ti in range(GT)]
        while pending:
            tail_tile(*pending.pop(0))
        seq["scan_peak"] = SB.lo
        P.barrier()
        SB.release(mB)

    def phase_ffn(l, lay, seq, experts, nff, h_dst, final):
        NT = seq["NT"]
        var = seq["var"]
        scr = seq["scr"]
        TB = min(1024, NT)
        nTB = NT // TB
        SUBW = min(512, TB)
        nsub = TB // SUBW
        nts = TB // 128
        NJ = nff // 128
        mF = SB.mark()
        u2 = SB.alloc([128, 8, TB], BF16)
        act = SB.alloc([128, NJ, TB], BF16)
        yacc = SB.alloc([128, nts, D], F32)
        wds = Rot([SB.alloc([128, NJ, 512], BF16) for _ in range(2)])
        WB = 128
        wgs = Rot([SB.alloc([128, 8, WB], BF16) for _ in range(2)])
        wus = Rot([SB.alloc([128, 8, WB], BF16) for _ in range(2)])
        sgs = Rot([SB.alloc([128, SUBW], BF16) for _ in range(2)])
        hos = Rot([SB.alloc([128, D], F32) for _ in range(2)])
        gt2 = lay["gtb"]["gt2" + ("x" if var == 0 else "c")]
        if final:
            gfin = SB.alloc([128, D], F32)
            LD(gfin[:], gfin_in, gfin)
            tmpf = Rot([(SB.alloc([128, 2], F32), SB.alloc([128, D], F32)) for _ in range(1)])
        moeg = seq.get("moeg")
        def residual_tile(T0, ts):
            t = T0 // 128 + ts
            ho = hos.next()
            LD(ho[:], scr["h1"][t * 128:(t + 1) * 128, :], ho, reads=[B("h1", var)])
            ybs = [B("yacc", ts, 0), B("yacc", ts, 1)]
            TT_("dve", yacc[:, ts, :], yacc[:, ts, :], gt2[:], ALU.mult, [gt2.b], ybs)
            TT_("dve", ho[:], ho[:], yacc[:, ts, :], ALU.add, ybs, [ho.b])
            if not final:
                ST(h_dst[0][t * 128:(t + 1) * 128, :], ho[:], ho, h_dst[1])
            else:
                ss, xn = tmpf.next()
                ACT(xn[:], ho[:], AF.Square, [ho.b], [xn.b, ss.b], accum_out=ss[:, 0:1])
                ACT(ss[:, 1:2], ss[:, 0:1], AF.Sqrt, [epsc.b], [ss.b], scale=1.0 / D, bias=epsc[:, 0:1])
                P.op("dve", lambda e, ss=ss: e.reciprocal(out=ss[:, 1:2], in_=ss[:, 1:2]), [], [ss.b])
                STT("dve", xn[:], ho[:], ss[:, 1:2], gfin[:], ALU.mult, ALU.mult, [ho.b, ss.b, gfin.b], [xn.b])
                ST(y_out[t * 128:(t + 1) * 128, :], xn[:], xn, B("y"))

        def load_u2(tb_):
            LD(u2[:], scr["u2T"][:, tb_ * TB:(tb_ + 1) * TB].rearrange("(c p) t -> p c t", p=128), u2, reads=[B("u2T", var)])

        load_u2(0)
        pend_res = []
        for tb in range(nTB):
            T0 = tb * TB
            for ei, (wg_ap, wu_ap, wd_ap) in enumerate(experts):
                wd0 = None
                for j0 in range(0, nff, WB):
                    if j0 == 2 * WB:
                        wd0 = wds.next()
                        LD(wd0[:], wd_ap[:, 0:512].rearrange("(j p) n -> p j n", p=128), wd0, q="pool")
                    ncol = min(WB, nff - j0)
                    wg, wu = wgs.next(), wus.next()
                    LD(wg[:, :, 0:ncol], wg_ap[:, j0:j0 + ncol].rearrange("(k p) n -> p k n", p=128), wg, q="pool")
                    LD(wu[:, :, 0:ncol], wu_ap[:, j0:j0 + ncol].rearrange("(k p) n -> p k n", p=128), wu, q="pool")
                    for c in range(ncol // 128):
                        j = j0 // 128 + c
                        for s in range(nsub):
                            bg = bankrot.next()
                            for k in range(8):
                                MM(bg, bg[:, 0:SUBW], wg[:, k, c * 128:(c + 1) * 128], u2[:, k, s * SUBW:(s + 1) * SUBW], k == 0,
                                   k == 7, [wg.b, u2.b])
                            bu = bankrot.next()
                            for k in range(8):
                                MM(bu, bu[:, 0:SUBW], wu[:, k, c * 128:(c + 1) * 128], u2[:, k, s * SUBW:(s + 1) * SUBW], k == 0,
                                   k == 7, [wu.b, u2.b])
                            sg = sgs.next()
                            ACT(sg[:], bg[:, 0:SUBW], AF.Silu, [], [bg.b, sg.b])
                            TT_("dve", act[:, j, s * SUBW:(s + 1) * SUBW], bu[:, 0:SUBW], sg[:], ALU.mult, [sg.b], [bu.b, act.b])
                    if pend_res and (j0 // WB) % 2 == 1:
                        residual_tile(*pend_res.pop(0))
                while pend_res:
                    residual_tile(*pend_res.pop(0))
                if ei == len(experts) - 1 and tb + 1 < nTB:
                    load_u2(tb + 1)
                for hf in range(2):
                    if hf == 0:
                        wd = wd0
                    else:
                        wd = wds.next()
                        LD(wd[:], wd_ap[:, hf * 512:(hf + 1) * 512].rearrange("(j p) n -> p j n", p=128), wd, q="pool")
                    for ts in range(nts):
                        bank = bankrot.next()
                        for j in range(NJ):
                            MM(bank, bank[:, :], act[:, j, ts * 128:(ts + 1) * 128], wd[:, j, :], j == 0, j == NJ - 1, [act.b, wd.b])
                        ya = yacc[:, ts, hf * 512:(hf + 1) * 512]
                        yb = B("yacc", ts, hf)
                        if moeg is None:
                            CP("act", ya, bank[:, :], [], [bank.b, yb])
                        else:
                            tg = (T0 // 128) + ts
                            if ei == 0:
                                ACT(ya, bank[:, :], AF.Identity, [moeg.b], [bank.b, yb], scale=moeg[:, tg, ei:ei + 1])
                            else:
                                STT("dve", ya, bank[:, :], moeg[:, tg, ei:ei + 1], ya, ALU.mult, ALU.add, [moeg.b], [bank.b, yb])
            pend_res = [(T0, ts) for ts in range(nts)]
        while pend_res:
            residual_tile(*pend_res.pop(0))
        P.barrier()
        SB.release(mF)

    stop = debug.get("stop")
    hx_src, hx_buf = x_in, B("h_in_x")
    hc_src, hc_buf = ctx_in, B("h_in_c")
    for l in range(2):
        last = l == 1
        mL = SB.mark()
        lay = adaln(l, want_ctx_gt=not last)
        moeg_t = SB.alloc([128, SEQ // 128, NE], F32) if last else None
        mM = SB.mark()
        Cf = SB.alloc([128, 8, 2, 257], F32)
        Cb = SB.alloc([128, 8, 2, 257], BF16)
        MS("dve", Cf[:], 0.0, [B("Cf", i) for i in range(8)])
        MS("dve", Cb[:], 0.0, [B("Cb", i) for i in range(8)])

        def mkseq(NT, var, scr, with_out, hbuf):
            TT = NT // 128
            s = dict(NT=NT, var=var, scr=scr, with_out=with_out, hbuf=hbuf, Cf=Cf, Cb=Cb)
            s["gsb"] = SB.alloc([128, TT, 16], F32)
            s["gd"] = {n: SB.alloc([128, TT, 8], F32) for n in ("lf", "bc", "a_s", "wk", "ebl")}
            return s

        sc = mkseq(CTX, 1, scr_c, not last, hc_buf)
        sx = mkseq(SEQ, 0, scr_x, True, hx_buf)
        if last:
            sx["moeg"] = moeg_t
        if not debug.get("skip_ctx"):
            phase_proj(l, lay, sc, hc_src)
            phase_scan(l, lay, sc, hc_src, False)
        if stop == ("ctx_scan", l):
            break
        phase_proj(l, lay, sx, hx_src)
        if stop == ("proj", l):
            break
        phase_scan(l, lay, sx, hx_src, last)
        if stop == ("scan", l):
            break
        P.barrier()
        SB.release(mM)
        if not last:
            dense = [(w_ffg[0], w_ffu[0], w_ffd[0])]
            phase_ffn(l, lay, sx, dense, DFF, (scr_x["h2"], B("h2", 0)), False)
            phase_ffn(l, lay, sc, dense, DFF, (scr_c["h2"], B("h2", 1)), False)
            hx_src, hx_buf = scr_x["h2"], B("h2", 0)
            hc_src, hc_buf = scr_c["h2"], B("h2", 1)
        else:
            experts = [(w_eg[0, e], w_eu[0, e], w_ed[0, e]) for e in range(NE)]
            phase_ffn(l, lay, sx, experts, DFE, None, True)
        P.barrier()
        SB.release(mL)
    stats = P.finalize()
    return nc, stats


def _consts():
    i = np.arange(128)
    ident = np.eye(128, dtype=np.float32)
    triF = (i[:, None] <= i[None, :]).astype(np.float32)
    triB = (i[:, None] >= i[None, :]).astype(np.float32)
    negF = np.where(i[:, None] <= i[None, :], 0.0, NEG).astype(np.float32)
    negB = np.where(i[:, None] >= i[None, :], 0.0, NEG).astype(np.float32)
    ones = np.ones((128, 128), np.float32)
    cmat = np.stack([ident, triF, triB, negF, negB, ones]).astype(np.float32)

    def cnt(n, w):
        t = np.arange(n)
        lo = np.clip(t - w // 2, 0, n)
        hi = np.clip(t + w // 2, 0, n)
        return (hi - lo).astype(np.float32)

    lat = []
    cx = []
    for w in POOL_W:
        c1 = cnt(64, w)
        lat.append(np.broadcast_to((1.0 / (c1[:, None] * c1[None, :])).reshape(1, SEQ), (128, SEQ)))
        cx.append(np.broadcast_to((1.0 / cnt(CTX, w)).reshape(1, CTX), (128, CTX)))
    return cmat, np.ascontiguousarray(np.stack(lat), np.float32), np.ascontiguousarray(np.stack(cx), np.float32)


def _pk(v, k):
    return np.ascontiguousarray(np.asarray(v, np.float32).reshape(k, 128).T)


def prep_inputs(inputs):
    f = lambda a: np.ascontiguousarray(np.asarray(a, dtype=np.float32))
    x, c, ctx, c_ctx = f(inputs["x"]), f(inputs["c"]), f(inputs["ctx"]), f(inputs["c_ctx"])
    w_in = f(inputs["w_in"])
    cmat, invc_lat, invc_ctx = _consts()
    perm = [d * 8 + g * 4 + h for g in range(2) for d in range(2) for h in range(4)]
    w_if = np.ascontiguousarray(w_in[:, :, IF_OFF:IF_OFF + 16][:, :, perm])
    b_if = np.ascontiguousarray(f(inputs["b_if"]).transpose(0, 2, 1, 3).reshape(2, 1, 16))
    vecs = np.stack([np.concatenate([_pk(inputs["g_mix"][l], 8), _pk(inputs["head_gain"][l], 8), _pk(inputs["g_ffn"][l], 8),
                                     _pk(inputs["pool_scale"][l], 4)], axis=1) for l in range(2)])
    convw = np.stack([np.ascontiguousarray(f(inputs["conv_qk"])[l].reshape(3, 16, 128).transpose(2, 1, 0)) for l in range(2)])
    shared = {
        "w_ada": f(inputs["w_ada"]), "b_ada": f(inputs["b_ada"]).reshape(2, 1, 6 * D),
        "vecs": np.ascontiguousarray(vecs, np.float32), "convw": np.ascontiguousarray(convw, np.float32),
        "w_in": w_in, "w_if": w_if, "b_if": b_if,
        "w_pool": f(inputs["w_pool"]), "w_pa": f(inputs["w_pa"]), "w_pb": f(inputs["w_pb"]), "w_out": f(inputs["w_out"]),
        "w_ff_gate": f(inputs["w_ff_gate"]), "w_ff_up": f(inputs["w_ff_up"]), "w_ff_down": f(inputs["w_ff_down"]),
        "w_router": f(inputs["w_router"]), "w_exp_gate": f(inputs["w_exp_gate"]), "w_exp_up": f(inputs["w_exp_up"]),
        "w_exp_down": f(inputs["w_exp_down"]),
        "g_final_b": np.ascontiguousarray(np.broadcast_to(f(inputs["g_final"])[None, :], (128, D))),
        "cmat": cmat, "invc_lat": invc_lat, "invc_ctx": invc_ctx,
    }
    in_maps = []
    for b in range(8):
        m = dict(shared)
        m["x"] = x[b]
        m["ctx"] = ctx[b]
        m["cc"] = np.ascontiguousarray(np.stack([_pk(c[b], 8), _pk(c_ctx, 8)], axis=-1))
        in_maps.append(m)
    return in_maps


def kernel(**inputs):
    in_maps = prep_inputs(inputs)
    nc, _ = build()
    res = run_bass_kernel_spmd(nc, in_maps, core_ids=list(range(8)))
    return np.stack([np.asarray(r["y"], dtype=np.float32) for r in res.results], axis=0)
```
